# Optimizing a Trainium2 kernel written in Bass

```python
import math
import jax
import jax.numpy as jnp
from jax import lax
import numpy as np

D_MODEL = 2048
BATCH = 4
SEQ = 4096
DEPTH = 4

CTX_LEN = 256
GRID_W = 64
N_MIXERS = 3
NORM_EPS = 1e-6
ROPE_THETA = 10000.0

SWA_HEADS = 32
SWA_KV_HEADS = 4
SWA_HEAD_DIM = 64
WINDOW = 128
WIN_BLOCK = 128

SSD_INNER = 2 * D_MODEL
SSD_HEAD_DIM = 64
SSD_HEADS = SSD_INNER // SSD_HEAD_DIM
SSD_GROUPS = 8
SSD_STATE = 128
SSD_CONV = 5
SSD_CHUNK = 128
SSD_CONV_DIM = SSD_INNER + 2 * SSD_GROUPS * SSD_STATE

GA_HEADS = 16
GA_KV_HEADS = 4
GA_HEAD_DIM = 128
GA_Q_BLOCK = 128

N_EXPERTS = 32
TOP_K = 4
EXPERT_FF = 896
SWIGLU_ALPHA = 1.702
SWIGLU_LIMIT = 7.0
MOE_BLOCK = 256

kernel_name = 'hybrid_interleaved_swa_ssd_axialattn_moe_dit'

F32 = jnp.float32


def rms_norm(x, g):
    xf = x.astype(F32)
    y = xf * lax.rsqrt(jnp.mean(xf * xf, axis=-1, keepdims=True) + NORM_EPS)
    return (y * g.astype(F32)).astype(x.dtype)


def modulate(h, shift, scale):
    return h * (1.0 + scale) + shift


def axial_rope_tables(pos_row, pos_col, head_dim):
    q4 = head_dim // 4
    inv = ROPE_THETA ** (-jnp.arange(q4, dtype=F32) / q4)
    ang = jnp.stack([pos_row.astype(F32)[:, None] * inv, pos_col.astype(F32)[:, None] * inv], axis=1)
    return jnp.cos(ang), jnp.sin(ang)


def apply_rope(x, cos, sin):
    lead = x.shape[:-1]
    q4 = x.shape[-1] // 4
    xr = x.astype(F32).reshape(lead + (2, 2, q4))
    x1, x2 = xr[..., 0, :], xr[..., 1, :]
    bshape = (cos.shape[0],) + (1,) * (x.ndim - 3) + cos.shape[1:]
    cs, sn = cos.reshape(bshape), sin.reshape(bshape)
    out = jnp.stack([x1 * cs - x2 * sn, x2 * cs + x1 * sn], axis=-2)
    return out.reshape(x.shape).astype(x.dtype)


def qkv_project(h, w_in, q_gain, k_gain, n_q, n_kv, hd, rope):
    bsz, n, _ = h.shape
    q, k, v = jnp.split(h @ w_in, [n_q * hd, (n_q + n_kv) * hd], axis=-1)
    q = rms_norm(q.reshape(bsz, n, n_kv, n_q // n_kv, hd), q_gain)
    k = rms_norm(k.reshape(bsz, n, n_kv, hd), k_gain)
    v = v.reshape(bsz, n, n_kv, hd)
    if rope is not None:
        q, k = apply_rope(q, *rope), apply_rope(k, *rope)
    return q * (hd ** -0.5), k, v


def attend(q, keys, vals, mask=None, sink=None):
    logits = [jnp.einsum('bqkrd,blkd->bkrql', q, k).astype(F32) for k in keys]
    if mask is not None:
        logits[0] = jnp.where(mask, logits[0], -jnp.inf)
    if sink is not None:
        logits.append(jnp.broadcast_to(sink.astype(F32)[None, :, :, None, None], logits[0].shape[:-1] + (1,)))
    cuts = [int(s) for s in np.cumsum([l.shape[-1] for l in logits])[:-1]]
    probs = jnp.split(jax.nn.softmax(jnp.concatenate(logits, axis=-1), axis=-1), cuts, axis=-1)
    return sum(jnp.einsum('bkrql,blkd->bqkrd', p.astype(v.dtype), v) for p, v in zip(probs, vals))


def window_attention(h_c, h_l, w_in, q_gain, k_gain, sink, w_out, rope, need_ctx):
    bsz, seq, _ = h_l.shape
    qc, kc, vc = qkv_project(h_c, w_in, q_gain, k_gain, SWA_HEADS, SWA_KV_HEADS, SWA_HEAD_DIM, None)
    ql, kl, vl = qkv_project(h_l, w_in, q_gain, k_gain, SWA_HEADS, SWA_KV_HEADS, SWA_HEAD_DIM, rope)
    sink = sink.reshape(SWA_KV_HEADS, SWA_HEADS // SWA_KV_HEADS)
    nb = seq // WIN_BLOCK
    span = WIN_BLOCK + 2 * WINDOW
    pad = ((0, 0), (WINDOW, WINDOW), (0, 0), (0, 0))
    k_pad, v_pad = jnp.pad(kl, pad), jnp.pad(vl, pad)
    q_blocks = jnp.moveaxis(ql.reshape((bsz, nb, WIN_BLOCK) + ql.shape[2:]), 1, 0)
    offs_q = jnp.arange(WIN_BLOCK)
    offs_k = jnp.arange(span) - WINDOW

    def one_block(args):
        qb, start = args
        kw = lax.dynamic_slice_in_dim(k_pad, start, span, axis=1)
        vw = lax.dynamic_slice_in_dim(v_pad, start, span, axis=1)
        qpos, kpos = start + offs_q, start + offs_k
        band = (jnp.abs(qpos[:, None] - kpos[None, :]) <= WINDOW) & ((kpos >= 0) & (kpos < seq))[None, :]
        return attend(qb, [kw, kc], [vw, vc], mask=band, sink=sink)

    y_l = lax.map(one_block, (q_blocks, jnp.arange(nb) * WIN_BLOCK))
    y_l = jnp.moveaxis(y_l, 0, 1).reshape(bsz, seq, -1) @ w_out
    y_c = None
    if need_ctx:
        y_c = attend(qc, [kc], [vc], sink=sink).reshape(bsz, h_c.shape[1], -1) @ w_out
    return y_c, y_l


def global_attention(h_c, h_l, w_in, q_gain, k_gain, w_out, rope, need_ctx):
    bsz, seq, _ = h_l.shape
    qc, kc, vc = qkv_project(h_c, w_in, q_gain, k_gain, GA_HEADS, GA_KV_HEADS, GA_HEAD_DIM, None)
    ql, kl, vl = qkv_project(h_l, w_in, q_gain, k_gain, GA_HEADS, GA_KV_HEADS, GA_HEAD_DIM, rope)
    nb = seq // GA_Q_BLOCK
    q_blocks = jnp.moveaxis(ql.reshape((bsz, nb, GA_Q_BLOCK) + ql.shape[2:]), 1, 0)
    y_l = lax.map(lambda qb: attend(qb, [kl, kc], [vl, vc]), q_blocks)
    y_l = jnp.moveaxis(y_l, 0, 1).reshape(bsz, seq, -1) @ w_out
    y_c = None
    if need_ctx:
        y_c = attend(qc, [kc], [vc]).reshape(bsz, h_c.shape[1], -1) @ w_out
    return y_c, y_l


def segsum(a):
    t = a.shape[-1]
    strict = jnp.tril(jnp.ones((t, t), bool), -1)
    incl = jnp.tril(jnp.ones((t, t), bool), 0)
    cs = jnp.cumsum(jnp.where(strict, a[..., :, None], 0.0), axis=-2)
    return jnp.where(incl, cs, -jnp.inf)


def ssd_scan(x, dt, a, bm, cm, init_state, with_output):
    bsz, n, n_h, p = x.shape
    g, ns = bm.shape[2], bm.shape[3]
    r = n_h // g
    nc = n // SSD_CHUNK
    xdt = (x * dt[..., None]).reshape(bsz, nc, SSD_CHUNK, g, r, p)
    adt = (dt * a).reshape(bsz, nc, SSD_CHUNK, g, r).transpose(0, 3, 4, 1, 2)
    bm = bm.reshape(bsz, nc, SSD_CHUNK, g, ns)
    cm = cm.reshape(bsz, nc, SSD_CHUNK, g, ns)
    a_cs = jnp.cumsum(adt, axis=-1)
    decay_states = jnp.exp(a_cs[..., -1:] - a_cs)
    states = jnp.einsum('bclgn,bgrcl,bclgrp->bcgrpn', bm, decay_states, xdt)
    states = jnp.concatenate([init_state[:, None], states], axis=1)
    chunk_tot = jnp.pad(a_cs[..., -1], ((0, 0), (0, 0), (0, 0), (1, 0)))
    decay_chunk = jnp.exp(segsum(chunk_tot))
    new_states = jnp.einsum('bgrzc,bcgrpn->bzgrpn', decay_chunk, states)
    final = new_states[:, -1]
    if not with_output:
        return None, final
    cb = jnp.einsum('bclgn,bcsgn->bcgls', cm, bm)
    lmat = jnp.exp(segsum(adt))
    y_diag = jnp.einsum('bcgls,bgrcls,bcsgrp->bclgrp', cb, lmat, xdt)
    y_off = jnp.einsum('bclgn,bcgrpn,bgrcl->bclgrp', cm, new_states[:, :-1], jnp.exp(a_cs))
    return (y_diag + y_off).reshape(bsz, n, n_h, p), final


def centred_dwconv(u, w, b):
    k, ch = w.shape
    out = lax.conv_general_dilated(u, w[:, None, :].astype(u.dtype), window_strides=(1,),
                                   padding=[(k // 2, k // 2)], dimension_numbers=('NWC', 'WIO', 'NWC'),
                                   feature_group_count=ch)
    return out + b


def bidir_ssd(h_c, h_l, w_in, conv_w, conv_b, dt_bias, a_log, d_skip, norm_g, w_out, need_ctx):
    a = -jnp.exp(a_log.astype(F32))

    def prepare(h):
        bsz, n, _ = h.shape
        z, xbc, dt = jnp.split(h @ w_in, [SSD_INNER, SSD_INNER + SSD_CONV_DIM], axis=-1)
        xbc = jax.nn.silu(centred_dwconv(xbc, conv_w, conv_b))
        xs, bm, cm = jnp.split(xbc, [SSD_INNER, SSD_INNER + SSD_GROUPS * SSD_STATE], axis=-1)
        dt = jax.nn.softplus(dt.astype(F32).reshape(bsz, n, 2, SSD_HEADS) + dt_bias.astype(F32))
        return (z, xs.reshape(bsz, n, SSD_HEADS, SSD_HEAD_DIM).astype(F32),
                bm.reshape(bsz, n, SSD_GROUPS, SSD_STATE).astype(F32),
                cm.reshape(bsz, n, SSD_GROUPS, SSD_STATE).astype(F32), dt)

    def finish(y, z):
        bsz, n = y.shape[:2]
        y = y.reshape(bsz, n, SSD_INNER) * jax.nn.silu(z.astype(F32))
        y = rms_norm(y.reshape(bsz, n, SSD_GROUPS, -1), norm_g.reshape(SSD_GROUPS, -1))
        return y.reshape(bsz, n, SSD_INNER).astype(h_l.dtype) @ w_out

    zc, xc, bc, cc, dtc = prepare(h_c)
    zl, xl, bl, cl, dtl = prepare(h_l)
    d_skip = d_skip.astype(F32)[:, None]
    y_l = d_skip * xl
    y_c = d_skip * xc if need_ctx else None
    bsz = h_l.shape[0]
    r = SSD_HEADS // SSD_GROUPS
    for direction in range(2):
        f = (lambda t: jnp.flip(t, axis=1)) if direction else (lambda t: t)
        init = jnp.zeros((bsz, SSD_GROUPS, r, SSD_HEAD_DIM, SSD_STATE), F32)
        yc_d, ctx_state = ssd_scan(f(xc), f(dtc[:, :, direction]), a[direction], f(bc), f(cc), init, need_ctx)
        yl_d, _ = ssd_scan(f(xl), f(dtl[:, :, direction]), a[direction], f(bl), f(cl), ctx_state, True)
        y_l = y_l + f(yl_d)
        if need_ctx:
            y_c = y_c + f(yc_d)
    return (finish(y_c, zc) if need_ctx else None), finish(y_l, zl)


def moe_ffn(h, w_router, b_router, w_gu, b_gu, w_down, b_down):
    n_tok, d = h.shape
    n_exp = w_router.shape[-1]
    n_asg = n_tok * TOP_K
    logits = (h @ w_router + b_router).astype(F32)
    top_val, top_idx = lax.top_k(logits, TOP_K)
    gates = jax.nn.softmax(top_val, axis=-1)
    e_flat = top_idx.reshape(-1)
    order = jnp.argsort(e_flat)
    e_sorted = e_flat[order]
    tok_sorted = order // TOP_K
    gate_sorted = gates.reshape(-1)[order]
    counts = jnp.bincount(e_flat, length=n_exp)
    padded = (counts + MOE_BLOCK - 1) // MOE_BLOCK * MOE_BLOCK
    pad_end = jnp.cumsum(padded)
    pad_start = pad_end - padded
    grp_start = jnp.cumsum(counts) - counts
    dest = pad_start[e_sorted] + jnp.arange(n_asg) - grp_start[e_sorted]
    n_blocks = -(-n_asg // MOE_BLOCK) + n_exp
    rows = jnp.zeros((n_blocks * MOE_BLOCK, d), h.dtype).at[dest].set(h[tok_sorted])
    block_exp = jnp.minimum(jnp.searchsorted(pad_end, jnp.arange(n_blocks) * MOE_BLOCK, side='right'), n_exp - 1)

    def expert_block(args):
        xb, e = args
        gu = xb @ w_gu[e] + b_gu[e]
        glu, lin = jnp.split(gu, 2, axis=-1)
        glu = jnp.minimum(glu, SWIGLU_LIMIT)
        lin = jnp.clip(lin, -SWIGLU_LIMIT, SWIGLU_LIMIT)
        act = glu * jax.nn.sigmoid(SWIGLU_ALPHA * glu) * (lin + 1.0)
        return act @ w_down[e] + b_down[e]

    out_rows = lax.map(expert_block, (rows.reshape(n_blocks, MOE_BLOCK, d), block_exp)).reshape(-1, d)
    y = out_rows[dest] * gate_sorted[:, None].astype(out_rows.dtype)
    return jax.ops.segment_sum(y, tok_sorted, num_segments=n_tok)


def setup_inputs(seed: int = 0) -> dict:
    key = jax.random.key(seed)
    keys = iter(jax.random.split(key, 48))

    def normal(shape, scale):
        return jax.random.normal(next(keys), shape, jnp.float32) * scale

    def gain(shape):
        return 1.0 + normal(shape, 0.02)

    d = D_MODEL
    n_a = len(range(0, DEPTH, N_MIXERS))
    n_b = len(range(1, DEPTH, N_MIXERS))
    n_c = len(range(2, DEPTH, N_MIXERS))
    swa_cols = (SWA_HEADS + 2 * SWA_KV_HEADS) * SWA_HEAD_DIM
    ga_cols = (GA_HEADS + 2 * GA_KV_HEADS) * GA_HEAD_DIM
    ssd_cols = SSD_INNER + SSD_CONV_DIM + 2 * SSD_HEADS
    dt0 = jnp.exp(jax.random.uniform(next(keys), (n_b, 2, SSD_HEADS), jnp.float32,
                                     minval=math.log(1e-3), maxval=math.log(1e-1)))
    a_log = jnp.log(jax.random.uniform(next(keys), (n_b, 2, SSD_HEADS), jnp.float32, minval=1.0, maxval=16.0))
    return {
        'x': normal((BATCH, SEQ, d), 1.0),
        'c': normal((BATCH, d), 1.0),
        'ctx': normal((BATCH, CTX_LEN, d), 1.0),
        'c_ctx': normal((d,), 1.0),
        'ada_w': normal((DEPTH, d, 6 * d), 0.5 * d ** -0.5),
        'ada_b': normal((DEPTH, 6 * d), 0.02),
        'norm_mix': gain((DEPTH, d)),
        'norm_ffn': gain((DEPTH, d)),
        'swa_w_in': normal((n_a, d, swa_cols), d ** -0.5),
        'swa_q_norm': gain((n_a, SWA_HEAD_DIM)),
        'swa_k_norm': gain((n_a, SWA_HEAD_DIM)),
        'swa_sink': normal((n_a, SWA_HEADS), 1.0),
        'swa_w_out': normal((n_a, SWA_HEADS * SWA_HEAD_DIM, d), (SWA_HEADS * SWA_HEAD_DIM) ** -0.5),
        'ssd_w_in': normal((n_b, d, ssd_cols), d ** -0.5),
        'ssd_conv_w': normal((n_b, SSD_CONV, SSD_CONV_DIM), SSD_CONV ** -0.5),
        'ssd_conv_b': normal((n_b, SSD_CONV_DIM), 0.02),
        'ssd_dt_bias': dt0 + jnp.log(-jnp.expm1(-dt0)),
        'ssd_a_log': a_log,
        'ssd_d': gain((n_b, SSD_HEADS)),
        'ssd_norm': gain((n_b, SSD_INNER)),
        'ssd_w_out': normal((n_b, SSD_INNER, d), SSD_INNER ** -0.5),
        'ga_w_in': normal((n_c, d, ga_cols), d ** -0.5),
        'ga_q_norm': gain((n_c, GA_HEAD_DIM)),
        'ga_k_norm': gain((n_c, GA_HEAD_DIM)),
        'ga_w_out': normal((n_c, GA_HEADS * GA_HEAD_DIM, d), (GA_HEADS * GA_HEAD_DIM) ** -0.5),
        'moe_w_router': normal((DEPTH, d, N_EXPERTS), d ** -0.5),
        'moe_b_router': normal((DEPTH, N_EXPERTS), 0.01),
        'moe_w_gate_up': normal((DEPTH, N_EXPERTS, d, 2 * EXPERT_FF), d ** -0.5),
        'moe_b_gate_up': normal((DEPTH, N_EXPERTS, 2 * EXPERT_FF), 0.02),
        'moe_w_down': normal((DEPTH, N_EXPERTS, EXPERT_FF, d), EXPERT_FF ** -0.5),
        'moe_b_down': normal((DEPTH, N_EXPERTS, d), 0.02),
    }


def reference(x, c, ctx, c_ctx, ada_w, ada_b, norm_mix, norm_ffn,
              swa_w_in, swa_q_norm, swa_k_norm, swa_sink, swa_w_out,
              ssd_w_in, ssd_conv_w, ssd_conv_b, ssd_dt_bias, ssd_a_log, ssd_d, ssd_norm, ssd_w_out,
              ga_w_in, ga_q_norm, ga_k_norm, ga_w_out,
              moe_w_router, moe_b_router, moe_w_gate_up, moe_b_gate_up, moe_w_down, moe_b_down):
    bsz, seq, d = x.shape
    n_ctx = ctx.shape[1]
    rows = seq // GRID_W
    pos_row = jnp.repeat(jnp.arange(rows), GRID_W)
    pos_col = jnp.tile(jnp.arange(GRID_W), rows)
    rope_swa = axial_rope_tables(pos_row, pos_col, SWA_HEAD_DIM)
    rope_ga = axial_rope_tables(pos_row, pos_col, GA_HEAD_DIM)
    act_c = jax.nn.silu(c)
    act_cc = jax.nn.silu(c_ctx)
    for i in range(DEPTH):
        kind, j = i % N_MIXERS, i // N_MIXERS
        need_ctx = i < DEPTH - 1
        mod_l = [m[:, None, :] for m in jnp.split(act_c @ ada_w[i] + ada_b[i], 6, axis=-1)]
        mod_c = jnp.split(act_cc @ ada_w[i] + ada_b[i], 6, axis=-1)
        h_l = modulate(rms_norm(x, norm_mix[i]), mod_l[0], mod_l[1])
        h_c = modulate(rms_norm(ctx, norm_mix[i]), mod_c[0], mod_c[1])
        if kind == 0:
            y_c, y_l = window_attention(h_c, h_l, swa_w_in[j], swa_q_norm[j], swa_k_norm[j], swa_sink[j],
                                        swa_w_out[j], rope_swa, need_ctx)
        elif kind == 1:
            y_c, y_l = bidir_ssd(h_c, h_l, ssd_w_in[j], ssd_conv_w[j], ssd_conv_b[j], ssd_dt_bias[j],
                                 ssd_a_log[j], ssd_d[j], ssd_norm[j], ssd_w_out[j], need_ctx)
        else:
            y_c, y_l = global_attention(h_c, h_l, ga_w_in[j], ga_q_norm[j], ga_k_norm[j], ga_w_out[j],
                                        rope_ga, need_ctx)
        x = x + mod_l[2] * y_l
        if need_ctx:
            ctx = ctx + mod_c[2] * y_c
        h_l = modulate(rms_norm(x, norm_ffn[i]), mod_l[3], mod_l[4])
        if need_ctx:
            h_c = modulate(rms_norm(ctx, norm_ffn[i]), mod_c[3], mod_c[4])
            tokens = jnp.concatenate([h_c, h_l], axis=1).reshape(-1, d)
            f = moe_ffn(tokens, moe_w_router[i], moe_b_router[i], moe_w_gate_up[i], moe_b_gate_up[i],
                        moe_w_down[i], moe_b_down[i]).reshape(bsz, n_ctx + seq, d)
            ctx = ctx + mod_c[5] * f[:, :n_ctx]
            x = x + mod_l[5] * f[:, n_ctx:]
        else:
            f = moe_ffn(h_l.reshape(-1, d), moe_w_router[i], moe_b_router[i], moe_w_gate_up[i],
                        moe_b_gate_up[i], moe_w_down[i], moe_b_down[i]).reshape(bsz, seq, d)
            x = x + mod_l[5] * f
    return x
```

```python
import math
from contextlib import ExitStack
import numpy as np
import concourse.bass as bass
import concourse.mybir as mybir
from concourse.bass_utils import run_bass_kernel_spmd

F32 = mybir.dt.float32
BF16 = mybir.dt.bfloat16
I32 = mybir.dt.int32
AF = mybir.ActivationFunctionType
ALU = mybir.AluOpType
AX = mybir.AxisListType

D = 2048
NCORES = 8
EPS = 1e-6
NDS = 12


class TK:
    def __init__(self, nc, es):
        self.nc = nc
        self.E = {"pe": nc.tensor, "act": nc.scalar, "dve": nc.vector, "pool": nc.gpsimd, "sp": nc.sync}
        self.sem = {k: es.enter_context(nc.semaphore("s_" + k)) for k in ["pe", "act", "dve", "pool"]}
        self.cnt = {k: 0 for k in self.sem}
        self.seen = {k: {} for k in self.E}
        self.last_w = {}
        self.readers = {}
        self.dsem = {}
        self.dn = {}
        for q in ["sp", "pool", "act"]:
            self.dsem[q] = [es.enter_context(nc.semaphore("d_%s%d" % (q, i))) for i in range(NDS)]
            self.dn[q] = 0
        self.allsems = dict(self.sem)
        for q in self.dsem:
            for i, s in enumerate(self.dsem[q]):
                self.allsems["d_%s%d" % (q, i)] = s

    def _wait(self, eng, key, val):
        if eng == "pe" and key == "pe":
            return
        if self.seen[eng].get(key, 0) >= val:
            return
        self.E[eng].wait_ge(self.allsems[key], val)
        self.seen[eng][key] = val

    def _sync(self, eng, reads, writes):
        deps = []
        for r in reads:
            if r in self.last_w:
                deps.append(self.last_w[r])
        for w in writes:
            if w in self.last_w:
                deps.append(self.last_w[w])
            for k, v in self.readers.get(w, {}).items():
                deps.append((k, v))
        for k, v in deps:
            self._wait(eng, k, v)

    def _mark(self, mark, reads, writes):
        k, v = mark
        for r in reads:
            d = self.readers.setdefault(r, {})
            d[k] = max(d.get(k, 0), v)
        for w in writes:
            self.last_w[w] = mark
            self.readers[w] = {}

    def op(self, eng, fn, reads=(), writes=()):
        self._sync(eng, reads, writes)
        ins = fn()
        self.cnt[eng] += 1
        ins.then_inc(self.sem[eng], 1)
        self._mark((eng, self.cnt[eng]), reads, writes)
        return ins

    def dma(self, q, out, in_, reads=(), writes=(), **kw):
        n = self.dn[q]
        idx = n % NDS
        key = "d_%s%d" % (q, idx)
        if n >= NDS:
            self._wait(q, key, 16 * (n // NDS))
        self._sync(q, reads, writes)
        ins = self.E[q].dma_start(out=out, in_=in_, **kw)
        ins.then_inc(self.dsem[q][idx], 16)
        self.dn[q] = n + 1
        self._mark((key, 16 * (n // NDS + 1)), reads, writes)
        return ins

    def finish(self):
        for q in self.dsem:
            n = self.dn[q]
            for idx in range(min(n, NDS)):
                cntq = (n - idx + NDS - 1) // NDS
                self._wait("sp", "d_%s%d" % (q, idx), 16 * cntq)


def _run(nc, in_maps):
    res = run_bass_kernel_spmd(nc, in_maps, core_ids=list(range(NCORES)))
    return res.results


def build_gemm(T, K, N, mode, n0=0, bias=False, NB=512, emit_hT=False, out_bf16=False):
    nc = bass.Bass("TRN2", target_bir_lowering=False)
    nt = T // 128
    kc = K // 128
    nb = (N + NB - 1) // NB
    if mode == "resid":
        xT = nc.dram_tensor("xT", [K, T], BF16, kind="ExternalInput").ap()
        xres = nc.dram_tensor("xres", [T, N], F32, kind="ExternalInput").ap()
        modrow = nc.dram_tensor("modrow", [2, N], F32, kind="ExternalInput").ap()
    else:
        x = nc.dram_tensor("x", [T, K], F32, kind="ExternalInput").ap()
    if mode == "norm":
        AB = nc.dram_tensor("AB", [2, 3, K], F32, kind="ExternalInput").ap()
    w = nc.dram_tensor("w", [K, N], F32, kind="ExternalInput").ap()
    if bias:
        brow = nc.dram_tensor("brow", [1, N], F32, kind="ExternalInput").ap()
    ODT = BF16 if out_bf16 else F32
    out = nc.dram_tensor("out", [T, N], ODT, kind="ExternalOutput").ap()
    if emit_hT:
        hTo = nc.dram_tensor("hTo", [K, T], BF16, kind="ExternalOutput").ap()
    with ExitStack() as es:
        tk = TK(nc, es)
        sb = lambda name, shape, dt: es.enter_context(nc.sbuf_tensor(name, shape, dt))
        hT = sb("hT", [128, kc, T], BF16)
        ident = sb("ident", [128, 128], F32)
        ps = [es.enter_context(nc.psum_tensor("ps%d" % i, [128, 512], F32)) for i in range(8)]
        wbuf = [sb("wb%d" % i, [128, kc, NB], BF16) for i in range(2)]
        ob = [sb("ob%d" % i, [128, NB], ODT) for i in range(3)]
        if mode == "resid":
            for c in range(kc):
                tk.dma("pool", hT[:, c, :], xT[c * 128:(c + 1) * 128, :], writes=[("hT", c)])
            mrow = sb("mrow", [128, 2, N], F32)
            for s in range(2):
                tk.dma("pool", mrow[:, s, :], modrow[s:s + 1, :].partition_broadcast(128), writes=[("mrow", s)])
            xr = [sb("xr%d" % i, [128, NB], F32) for i in range(3)]
        else:
            tk.op("pool", lambda: nc.gpsimd.memset(ident[:], 0.0), writes=["ident"])
            tk.op("pool", lambda: nc.gpsimd.affine_select(out=ident[:], in_=ident[:], pattern=[[-1, 128]],
                                                          compare_op=ALU.not_equal, fill=1.0, base=0,
                                                          channel_multiplier=1), reads=["ident"], writes=["ident"])
            xt = [sb("xt%d" % i, [128, K], F32) for i in range(2)]
            xs = [sb("xs%d" % i, [128, K], F32) for i in range(2)]
            ss = sb("ss", [128, 4], F32)
            if mode == "norm":
                abr = sb("abr", [128, 2, 3, K], F32)
                for s in range(2):
                    for j in range(3):
                        tk.dma("pool", abr[:, s, j, :], AB[s, j:j + 1, :].partition_broadcast(128),
                               writes=[("abr", s, j)])
                for s in range(2):
                    tk.op("dve", lambda s=s: nc.vector.scalar_tensor_tensor(
                        out=abr[:, s, 0, :], in0=abr[:, s, 1, :], scalar=1.0, in1=abr[:, s, 0, :],
                        op0=ALU.add, op1=ALU.mult),
                        reads=[("abr", s, 1), ("abr", s, 0)], writes=[("abr", s, 0)])
            for t in range(nt):
                b = t % 2
                tk.dma("sp", xt[b][:], x[t * 128:(t + 1) * 128, :], writes=[("xt", b)])
                if mode == "norm":
                    s = 0 if t < n0 else 1
                    tk.op("act", lambda b=b: nc.scalar.activation(out=xs[b][:], in_=xt[b][:], func=AF.Square,
                                                                 accum_out=ss[:, 0:1]),
                          reads=[("xt", b)], writes=[("xs", b), "ss0"])
                    tk.op("act", lambda: nc.scalar.activation(out=ss[:, 1:2], in_=ss[:, 0:1], func=AF.Sqrt,
                                                              scale=1.0 / K, bias=EPS), reads=["ss0"], writes=["ss1"])
                    tk.op("dve", lambda: nc.vector.reciprocal(out=ss[:, 2:3], in_=ss[:, 1:2]), reads=["ss1"],
                          writes=["ss2"])
                    tk.op("dve", lambda b=b, s=s: nc.vector.scalar_tensor_tensor(
                        out=xs[b][:], in0=xt[b][:], scalar=ss[:, 2:3], in1=abr[:, s, 0, :], op0=ALU.mult,
                        op1=ALU.mult), reads=[("xt", b), "ss2", ("abr", s, 0)], writes=[("xs", b)])
                    tk.op("pool", lambda b=b, s=s: nc.gpsimd.tensor_tensor(
                        out=xs[b][:], in0=xs[b][:], in1=abr[:, s, 2, :], op=ALU.add),
                        reads=[("xs", b), ("abr", s, 2)], writes=[("xs", b)])
                else:
                    tk.op("act", lambda b=b: nc.scalar.activation(out=xs[b][:], in_=xt[b][:], func=AF.Silu),
                          reads=[("xt", b)], writes=[("xs", b)])
                for c4 in range(kc // 4):
                    pi = c4 % 2
                    for j in range(4):
                        c = c4 * 4 + j
                        tk.op("pe", lambda b=b, c=c, j=j, pi=pi: nc.tensor.transpose(
                            out=ps[pi][:, j * 128:(j + 1) * 128], in_=xs[b][:, c * 128:(c + 1) * 128],
                            identity=ident[:]), reads=[("xs", b), "ident"], writes=[("ps", pi)])
                    eng = "act" if c4 % 2 == 0 else "dve"
                    dst = hT[:, c4 * 4:(c4 + 1) * 4, t * 128:(t + 1) * 128]
                    src = ps[pi][:].rearrange("p (j q) -> p j q", j=4)
                    if eng == "act":
                        tk.op("act", lambda dst=dst, src=src: nc.scalar.copy(out=dst, in_=src),
                              reads=[("ps", pi)], writes=[("hTt", t, c4)])
                    else:
                        tk.op("dve", lambda dst=dst, src=src: nc.vector.tensor_copy(out=dst, in_=src),
                              reads=[("ps", pi)], writes=[("hTt", t, c4)])
        if bias:
            bb = sb("bb", [128, N], F32)
            tk.dma("pool", bb[:], brow[0:1, :].partition_broadcast(128), writes=["bb"])
        if emit_hT:
            for c in range(kc):
                tk.dma("sp", hTo[c * 128:(c + 1) * 128, :], hT[:, c, :],
                       reads=[("hTt", t, c // 4) for t in range(nt)])
        hreads = ([("hT", c) for c in range(kc)] if mode == "resid"
                  else None)
        k = 0
        for n in range(nb):
            n0c = n * NB
            nw = min(NB, N - n0c)
            wb = n % 2
            tk.dma("pool", wbuf[wb][:, :, :nw], w[:, n0c:n0c + nw].rearrange("(c p) n -> p c n", p=128),
                   writes=[("wb", wb)], max_dma_last_dim=4096)
            for t in range(nt):
                pi = 2 + (k % 4)
                o = k % 3
                k += 1
                rd = hreads if hreads is not None else [("hTt", t, c4) for c4 in range(kc // 4)]
                for c in range(kc):
                    tk.op("pe", lambda c=c, t=t, pi=pi, wb=wb, nw=nw: nc.tensor.matmul(
                        ps[pi][:, :nw], lhsT=hT[:, c, t * 128:(t + 1) * 128], rhs=wbuf[wb][:, c, :nw],
                        start=(c == 0), stop=(c == kc - 1)), reads=rd + [("wb", wb)], writes=[("ps", pi)])
                if mode == "resid":
                    s = 0 if t < n0 else 1
                    tk.dma("sp", xr[o][:, :nw], xres[t * 128:(t + 1) * 128, n0c:n0c + nw], writes=[("xr", o)])
                    tk.op("dve", lambda pi=pi, o=o, s=s, nw=nw, n0c=n0c: nc.vector.tensor_tensor(
                        out=ob[o][:, :nw], in0=ps[pi][:, :nw], in1=mrow[:, s, n0c:n0c + nw], op=ALU.mult),
                        reads=[("ps", pi), ("mrow", s)], writes=[("ob", o)])
                    tk.op("pool", lambda o=o, nw=nw: nc.gpsimd.tensor_tensor(
                        out=ob[o][:, :nw], in0=ob[o][:, :nw], in1=xr[o][:, :nw], op=ALU.add),
                        reads=[("ob", o), ("xr", o)], writes=[("ob", o)])
                elif bias:
                    tk.op("dve", lambda pi=pi, o=o, nw=nw, n0c=n0c: nc.vector.tensor_tensor(
                        out=ob[o][:, :nw], in0=ps[pi][:, :nw], in1=bb[:, n0c:n0c + nw], op=ALU.add),
                        reads=[("ps", pi), "bb"], writes=[("ob", o)])
                else:
                    if k % 2 == 0:
                        tk.op("act", lambda pi=pi, o=o, nw=nw: nc.scalar.copy(out=ob[o][:, :nw], in_=ps[pi][:, :nw]),
                              reads=[("ps", pi)], writes=[("ob", o)])
                    else:
                        tk.op("dve", lambda pi=pi, o=o, nw=nw: nc.vector.tensor_copy(out=ob[o][:, :nw],
                                                                                      in_=ps[pi][:, :nw]),
                              reads=[("ps", pi)], writes=[("ob", o)])
                tk.dma("sp", out[t * 128:(t + 1) * 128, n0c:n0c + nw], ob[o][:, :nw], reads=[("ob", o)])
        tk.finish()
    return nc


NTOK = 4352
NTT = NTOK // 128


def build_attn(hd, R, window):
    nc = bass.Bass("TRN2", target_bir_lowering=False)
    KV = 2
    HQ = KV * R
    q4 = hd // 4
    G = R // 4
    q_in = nc.dram_tensor("q", [NTOK, HQ * hd], BF16, kind="ExternalInput").ap()
    k_in = nc.dram_tensor("k", [NTOK, KV * hd], BF16, kind="ExternalInput").ap()
    v_in = nc.dram_tensor("v", [NTOK, KV * hd], BF16, kind="ExternalInput").ap()
    gains = nc.dram_tensor("gains", [2, hd], F32, kind="ExternalInput").ap()
    cs_in = nc.dram_tensor("cossin", [NTOK, 2, hd // 2], F32, kind="ExternalInput").ap()
    masks = nc.dram_tensor("masks", [2, 128, 512], F32, kind="ExternalInput").ap()
    sink = nc.dram_tensor("sink", [1, HQ], F32, kind="ExternalInput").ap()
    yT = nc.dram_tensor("yT", [HQ * hd, NTOK], BF16, kind="ExternalOutput").ap()
    with ExitStack() as es:
        tk = TK(nc, es)
        sb = lambda name, shape, dt: es.enter_context(nc.sbuf_tensor(name, shape, dt))
        ps = [es.enter_context(nc.psum_tensor("ps%d" % i, [128, 512], F32)) for i in range(8)]
        ident = sb("ident", [128, 128], F32)
        tk.op("pool", lambda: nc.gpsimd.memset(ident[:], 0.0), writes=["ident"])
        tk.op("pool", lambda: nc.gpsimd.affine_select(out=ident[:], in_=ident[:], pattern=[[-1, 128]],
                                                      compare_op=ALU.not_equal, fill=1.0, base=0,
                                                      channel_multiplier=1), reads=["ident"], writes=["ident"])
        ones = sb("ones", [128, 128], BF16)
        tk.op("pool", lambda: nc.gpsimd.memset(ones[:], 1.0), writes=["ones"])
        gt = sb("gt", [128, 2, hd], F32)
        for j in range(2):
            tk.dma("pool", gt[:, j, :], gains[j:j + 1, :].partition_broadcast(128), writes=[("gt", j)])
        mk = sb("mk", [128, 2, 512], BF16)
        for j in range(2):
            tk.dma("pool", mk[:, j, :], masks[j], writes=[("mk", j)])
        esk = sb("esk", [128, HQ], F32)
        eskx = sb("eskx", [128, HQ, 128], F32)
        if window:
            tk.dma("pool", esk[:], sink[0:1, :].partition_broadcast(128), writes=["esk"])
            tk.op("act", lambda: nc.scalar.activation(out=esk[:], in_=esk[:], func=AF.Exp), reads=["esk"],
                  writes=["esk"])
            tk.op("dve", lambda: nc.vector.tensor_copy(out=eskx[:], in_=esk[:].unsqueeze(2).broadcast_to(
                [128, HQ, 128])), reads=["esk"], writes=["eskx"])
        kT = sb("kT", [128, KV, NTOK], BF16)
        V = sb("V", [128, NTT, KV * hd], BF16)
        cst = [sb("cst%d" % i, [128, 2, hd // 2], F32) for i in range(2)]
        W = max(HQ, KV) * hd
        xin = [sb("xin%d" % i, [128, W], F32) for i in range(2)]
        vin = [sb("vin%d" % i, [128, KV * hd], F32) for i in range(2)]
        tA = sb("tA", [128, W], F32)
        tB = sb("tB", [128, W], F32)
        tC = sb("tC", [128, W], F32)
        xr = sb("xr", [128, W], F32)
        st = sb("st", [128, 3, 32], F32)
        qT = [sb("qT%d" % i, [128, HQ, 128], BF16) for i in range(2)]
        Eb = [sb("Eb%d" % i, [128, 512], BF16) for i in range(3)]
        yo = [sb("yo%d" % i, [128, HQ, 128], F32) for i in range(2)]
        rd = sb("rd", [128, 512], F32)

        def normrope(src, H, gi, cs, scale, srckey):
            n = H * hd
            tk.op("act", lambda: nc.scalar.activation(out=tA[:, :n], in_=src, func=AF.Square),
                  reads=[srckey], writes=["tA"])
            tk.op("dve", lambda: nc.vector.tensor_reduce(out=st[:, 0, :H], in_=tA[:, :n].rearrange(
                "p (h d) -> p h d", h=H), axis=AX.X, op=ALU.add), reads=["tA"], writes=["st0"])
            tk.op("act", lambda: nc.scalar.activation(out=st[:, 1, :H], in_=st[:, 0, :H], func=AF.Sqrt,
                                                      scale=1.0 / hd, bias=EPS), reads=["st0"], writes=["st1"])
            tk.op("dve", lambda: nc.vector.reciprocal(out=st[:, 2, :H], in_=st[:, 1, :H]), reads=["st1"],
                  writes=["st2"])
            if scale != 1.0:
                tk.op("dve", lambda: nc.vector.tensor_scalar(out=st[:, 2, :H], in0=st[:, 2, :H], scalar1=scale,
                                                             scalar2=None, op0=ALU.mult), reads=["st2"],
                      writes=["st2"])
            tk.op("dve", lambda: nc.vector.tensor_tensor(
                out=tA[:, :n].rearrange("p (h d) -> p h d", h=H), in0=src.rearrange("p (h d) -> p h d", h=H),
                in1=st[:, 2, :H].unsqueeze(2).broadcast_to([128, H, hd]), op=ALU.mult),
                reads=[srckey, "st2"], writes=["tA"])
            tk.op("pool", lambda: nc.gpsimd.tensor_tensor(
                out=tA[:, :n].rearrange("p (h d) -> p h d", h=H), in0=tA[:, :n].rearrange("p (h d) -> p h d", h=H),
                in1=gt[:, gi, :].unsqueeze(1).broadcast_to([128, H, hd]), op=ALU.mult),
                reads=["tA", ("gt", gi)], writes=["tA"])
            v5 = lambda t: t[:, :n].rearrange("p (h a j q) -> p h a j q", h=H, a=2, j=2)
            x0 = v5(tA)[:, :, :, 0, :]
            x1 = v5(tA)[:, :, :, 1, :]
            C = cs[:, 0, :].rearrange("p (a q) -> p a q", a=2).unsqueeze(1).broadcast_to([128, H, 2, q4])
            S = cs[:, 1, :].rearrange("p (a q) -> p a q", a=2).unsqueeze(1).broadcast_to([128, H, 2, q4])
            b0 = v5(tB)[:, :, :, 0, :]
            b1 = v5(tB)[:, :, :, 1, :]
            c0 = v5(tC)[:, :, :, 0, :]
            c1 = v5(tC)[:, :, :, 1, :]
            o0 = v5(xr)[:, :, :, 0, :]
            o1 = v5(xr)[:, :, :, 1, :]
            tk.op("dve", lambda: nc.vector.tensor_tensor(out=b0, in0=x0, in1=C, op=ALU.mult),
                  reads=["tA", "cs"], writes=["tB0"])
            tk.op("pool", lambda: nc.gpsimd.tensor_tensor(out=c0, in0=x1, in1=S, op=ALU.mult),
                  reads=["tA", "cs"], writes=["tC0"])
            tk.op("dve", lambda: nc.vector.tensor_tensor(out=b1, in0=x1, in1=C, op=ALU.mult),
                  reads=["tA", "cs"], writes=["tB1"])
            tk.op("pool", lambda: nc.gpsimd.tensor_tensor(out=c1, in0=x0, in1=S, op=ALU.mult),
                  reads=["tA", "cs"], writes=["tC1"])
            tk.op("dve", lambda: nc.vector.tensor_tensor(out=o0, in0=b0, in1=c0, op=ALU.subtract),
                  reads=["tB0", "tC0"], writes=["xr0"])
            tk.op("pool", lambda: nc.gpsimd.tensor_tensor(out=o1, in0=b1, in1=c1, op=ALU.add),
                  reads=["tB1", "tC1"], writes=["xr1"])

        def load_cs(t):
            tk.dma("sp", cst[0][:], cs_in[t * 128:(t + 1) * 128], writes=["cs"])
            return cst[0]

        for t in range(NTT):
            b = t % 2
            tk.dma("pool", xin[b][:, :KV * hd], k_in[t * 128:(t + 1) * 128, :], writes=[("xin", b)])
            tk.dma("pool", vin[b][:], v_in[t * 128:(t + 1) * 128, :], writes=[("vin", b)])
            cs = load_cs(t)
            normrope(xin[b][:, :KV * hd], KV, 1, cs, 1.0, ("xin", b))
            for kv in range(KV):
                pi = kv
                tk.op("pe", lambda kv=kv, pi=pi: nc.tensor.transpose(
                    out=ps[pi][:hd, 0:128], in_=xr[:, kv * hd:(kv + 1) * hd], identity=ident[:]),
                    reads=["xr0", "xr1", "ident"], writes=[("ps", pi)])
                tk.op("act", lambda kv=kv, pi=pi, t=t: nc.scalar.copy(out=kT[:hd, kv, t * 128:(t + 1) * 128],
                                                                     in_=ps[pi][:hd, 0:128]),
                      reads=[("ps", pi)], writes=[("kT", t)])
            tk.op("act", lambda b=b, t=t: nc.scalar.copy(out=V[:, t, :], in_=vin[b][:]), reads=[("vin", b)],
                  writes=[("V", t)])

        for t in range(NTT):
            b = t % 2
            tk.dma("pool", xin[b][:, :HQ * hd], q_in[t * 128:(t + 1) * 128, :], writes=[("xin", b)])
            cs = load_cs(t)
            normrope(xin[b][:, :HQ * hd], HQ, 0, cs, hd ** -0.5, ("xin", b))
            for h in range(HQ):
                pi = h % 2
                tk.op("pe", lambda h=h, pi=pi: nc.tensor.transpose(
                    out=ps[pi][:hd, 0:128], in_=xr[:, h * hd:(h + 1) * hd], identity=ident[:]),
                    reads=["xr0", "xr1", "ident"], writes=[("ps", pi)])
                if h % 2 == 0:
                    tk.op("act", lambda h=h, pi=pi, b=b: nc.scalar.copy(out=qT[b][:hd, h, :], in_=ps[pi][:hd, 0:128]),
                          reads=[("ps", pi)], writes=[("qT", b, h)])
                else:
                    tk.op("dve", lambda h=h, pi=pi, b=b: nc.vector.tensor_copy(out=qT[b][:hd, h, :],
                                                                             in_=ps[pi][:hd, 0:128]),
                          reads=[("ps", pi)], writes=[("qT", b, h)])
            if t < 2:
                keys = [(0, None), (1, None)]
            elif not window:
                keys = [(j, None) for j in range(NTT)]
            else:
                keys = [(0, None), (1, None)]
                if t - 1 >= 2:
                    keys.append((t - 1, 0))
                keys.append((t, None))
                if t + 1 < NTT:
                    keys.append((t + 1, 1))
            it = 0
            for kv in range(KV):
                for g in range(G):
                    h0 = kv * R + g * 4
                    po = 4 + 2 * ((kv * G + g) % 2)
                    pd = po + 1
                    for ki, (kt, mi) in enumerate(keys):
                        pi = 2 + (it % 2)
                        e = it % 3
                        it += 1
                        tk.op("pe", lambda kv=kv, kt=kt, pi=pi, b=b, h0=h0: nc.tensor.matmul(
                            ps[pi][:, :], lhsT=kT[:hd, kv, kt * 128:(kt + 1) * 128],
                            rhs=qT[b][:hd, h0:h0 + 4, :].rearrange("p h q -> p (h q)"), start=True, stop=True),
                            reads=[("kT", kt)] + [("qT", b, h0 + i) for i in range(4)], writes=[("ps", pi)])
                        tk.op("act", lambda pi=pi, e=e: nc.scalar.activation(out=Eb[e][:], in_=ps[pi][:],
                                                                            func=AF.Exp),
                              reads=[("ps", pi)], writes=[("Eb", e)])
                        if mi is not None:
                            tk.op("dve", lambda e=e, mi=mi: nc.vector.tensor_tensor(
                                out=Eb[e][:], in0=Eb[e][:], in1=mk[:, mi, :], op=ALU.mult),
                                reads=[("Eb", e), ("mk", mi)], writes=[("Eb", e)])
                        first = ki == 0
                        last = ki == len(keys) - 1
                        tk.op("pe", lambda kv=kv, kt=kt, po=po, e=e, first=first, last=last: nc.tensor.matmul(
                            ps[po][:hd, :], lhsT=V[:, kt, kv * hd:(kv + 1) * hd], rhs=Eb[e][:], start=first,
                            stop=last), reads=[("V", kt), ("Eb", e)], writes=[("ps", po)])
                        tk.op("pe", lambda pd=pd, e=e, first=first, last=last: nc.tensor.matmul(
                            ps[pd][:hd, :], lhsT=ones[:, :hd], rhs=Eb[e][:], start=first, stop=last),
                            reads=["ones", ("Eb", e)], writes=[("ps", pd)])
                    if window:
                        tk.op("dve", lambda pd=pd, h0=h0: nc.vector.tensor_tensor(
                            out=rd[:hd, :], in0=ps[pd][:hd, :],
                            in1=eskx[:hd, h0:h0 + 4, :].rearrange("p h q -> p (h q)"), op=ALU.add),
                            reads=[("ps", pd), "eskx"], writes=["rd"])
                        tk.op("dve", lambda: nc.vector.reciprocal(out=rd[:hd, :], in_=rd[:hd, :]), reads=["rd"],
                              writes=["rd"])
                    else:
                        tk.op("dve", lambda pd=pd: nc.vector.reciprocal(out=rd[:hd, :], in_=ps[pd][:hd, :]),
                              reads=[("ps", pd)], writes=["rd"])
                    tk.op("dve", lambda po=po, b=b, h0=h0: nc.vector.tensor_tensor(
                        out=yo[b][:hd, h0:h0 + 4, :].rearrange("p h q -> p (h q)"), in0=ps[po][:hd, :],
                        in1=rd[:hd, :], op=ALU.mult), reads=[("ps", po), "rd"], writes=[("yo", b, h0)])
            tk.dma("pool", yT.rearrange("(h d) t -> d h t", d=hd)[:, :, t * 128:(t + 1) * 128], yo[b][:hd, :, :],
                   reads=[("yo", b, kv * R + g * 4) for kv in range(KV) for g in range(G)])
        tk.finish()
    return nc


NE = 32
FF = 896
NJ = FF // 128


def build_moe(T, n0, passes, dbg_ne=NE, stop=9):
    nc = bass.Bass("TRN2", target_bir_lowering=False)
    kc = D // 128
    x1 = nc.dram_tensor("x1", [T, D], F32, kind="ExternalInput").ap()
    ABt = nc.dram_tensor("ABt", [128, 2, 3, kc], F32, kind="ExternalInput").ap()
    mod5 = nc.dram_tensor("mod5", [2, D], F32, kind="ExternalInput").ap()
    wr_in = nc.dram_tensor("wr", [D, NE], F32, kind="ExternalInput").ap()
    br_in = nc.dram_tensor("br", [1, NE], F32, kind="ExternalInput").ap()
    wgu = nc.dram_tensor("wgu", [NE, D, 2 * FF], F32, kind="ExternalInput").ap()
    bguT = nc.dram_tensor("bguT", [128, NE, 2 * NJ], F32, kind="ExternalInput").ap()
    wdn = nc.dram_tensor("wdn", [NE, FF, D], F32, kind="ExternalInput").ap()
    bdn = nc.dram_tensor("bdn", [NE, D], F32, kind="ExternalInput").ap()
    out = nc.dram_tensor("out", [T, D], F32, kind="ExternalOutput").ap()
    maxp = max(passes)
    with ExitStack() as es:
        tk = TK(nc, es)
        sb = lambda name, shape, dt: es.enter_context(nc.sbuf_tensor(name, shape, dt))
        ps = [es.enter_context(nc.psum_tensor("ps%d" % i, [128, 512], F32)) for i in range(8)]
        ident = sb("ident", [128, 128], F32)
        tk.op("pool", lambda: nc.gpsimd.memset(ident[:], 0.0), writes=["ident"])
        tk.op("pool", lambda: nc.gpsimd.affine_select(out=ident[:], in_=ident[:], pattern=[[-1, 128]],
                                                      compare_op=ALU.not_equal, fill=1.0, base=0,
                                                      channel_multiplier=1), reads=["ident"], writes=["ident"])
        ab = sb("ab", [128, 2, 3, kc], F32)
        tk.dma("sp", ab[:], ABt, writes=["ab"])
        for s in range(2):
            tk.op("dve", lambda s=s: nc.vector.scalar_tensor_tensor(
                out=ab[:, s, 0, :], in0=ab[:, s, 1, :], scalar=1.0, in1=ab[:, s, 0, :], op0=ALU.add, op1=ALU.mult),
                reads=["ab"], writes=["ab"])
        mrow = sb("mrow", [128, 2, D], F32)
        for s in range(2):
            tk.dma("pool", mrow[:, s, :], mod5[s:s + 1, :].partition_broadcast(128), writes=[("mrow", s)])
        wr = sb("wr_sb", [128, kc, NE], F32)
        tk.dma("sp", wr[:], wr_in.rearrange("(c p) n -> p c n", p=128), writes=["wr"])
        brb = sb("brb", [128, NE], F32)
        tk.dma("pool", brb[:], br_in[0:1, :].partition_broadcast(128), writes=["brb"])
        bgu = sb("bgu", [128, NE, 2 * NJ], F32)
        tk.dma("sp", bgu[:], bguT, writes=["bgu"])
        bd = sb("bd", [128, D], BF16)
        tk.op("pool", lambda: nc.gpsimd.memset(bd[:], 0.0), writes=["bd"])
        tk.dma("pool", bd[:NE, :], bdn, writes=["bd"])
        hT = sb("hT", [128, kc, maxp * 128], BF16)
        acc = sb("acc", [128, maxp, D], F32)
        actT = sb("actT", [128, NJ, maxp * 128], BF16)
        Wd = sb("Wd", [128, NJ, D], BF16)
        wg = [sb("wg%d" % i, [128, kc, 2, 128], BF16) for i in range(2)]
        t1 = sb("t1", [128, 512], F32)
        t2 = sb("t2", [128, 512], F32)
        t3 = sb("t3", [128, 512], F32)
        xt = [sb("xt%d" % i, [128, D], F32) for i in range(2)]
        h32 = [sb("h32_%d" % i, [128, 4, 128], F32) for i in range(2)]
        G = sb("G", [128, maxp, 128], F32)
        tk.op("pool", lambda: nc.gpsimd.memset(G[:], 0.0), writes=[("G", i) for i in range(maxp)])
        GT = sb("GT", [128, 128], BF16)
        sm = sb("sm", [128, 64], F32)
        lg = sb("lg", [128, NE], F32)
        ex = sb("ex", [128, NE], F32)
        mk = sb("mk_sb", [128, NE], F32)

        tile0 = 0
        wi = 0
        for P in passes:
            ntok = P * 128
            for lt in range(P):
                t = tile0 + lt
                s = 0 if t < n0 else 1
                b = t % 2
                tk.dma("sp", xt[b][:], x1[t * 128:(t + 1) * 128, :], writes=[("xt", b)])
                if stop < 2:
                    continue
                tk.op("act", lambda b=b: nc.scalar.activation(out=t2[:, :], in_=xt[b][:, 0:512], func=AF.Square,
                                                             accum_out=sm[:, 8:9]), reads=[("xt", b)],
                      writes=["t2", "sm8"])
                tk.op("act", lambda b=b: nc.scalar.activation(out=t2[:, :], in_=xt[b][:, 512:1024], func=AF.Square,
                                                             accum_out=sm[:, 9:10]), reads=[("xt", b), "t2"],
                      writes=["t2", "sm9"])
                tk.op("act", lambda b=b: nc.scalar.activation(out=t2[:, :], in_=xt[b][:, 1024:1536], func=AF.Square,
                                                             accum_out=sm[:, 10:11]), reads=[("xt", b), "t2"],
                      writes=["t2", "sm10"])
                tk.op("act", lambda b=b: nc.scalar.activation(out=t2[:, :], in_=xt[b][:, 1536:2048], func=AF.Square,
                                                             accum_out=sm[:, 11:12]), reads=[("xt", b), "t2"],
                      writes=["t2", "sm11"])
                tk.op("dve", lambda: nc.vector.tensor_reduce(out=sm[:, 0:1], in_=sm[:, 8:12], axis=AX.X, op=ALU.add),
                      reads=["sm8", "sm9", "sm10", "sm11"], writes=["sm0"])
                tk.op("act", lambda: nc.scalar.activation(out=sm[:, 1:2], in_=sm[:, 0:1], func=AF.Sqrt,
                                                          scale=1.0 / D, bias=EPS), reads=["sm0"], writes=["sm1"])
                tk.op("dve", lambda: nc.vector.reciprocal(out=sm[:, 2:3], in_=sm[:, 1:2]), reads=["sm1"],
                      writes=["sm2"])
                tk.op("dve", lambda b=b: nc.vector.tensor_scalar(out=xt[b][:], in0=xt[b][:], scalar1=sm[:, 2:3],
                                                                 scalar2=None, op0=ALU.mult),
                      reads=[("xt", b), "sm2"], writes=[("xt", b)])
                pl = 7
                if stop < 3:
                    continue
                for c4 in range(kc // 4):
                    pi = c4 % 2
                    hb = c4 % 2
                    for j in range(4):
                        c = c4 * 4 + j
                        tk.op("pe", lambda b=b, c=c, j=j, pi=pi: nc.tensor.transpose(
                            out=ps[pi][:, j * 128:(j + 1) * 128], in_=xt[b][:, c * 128:(c + 1) * 128],
                            identity=ident[:]), reads=[("xt", b), "ident"], writes=[("ps", pi)])
                    for j in range(4):
                        c = c4 * 4 + j
                        tk.op("act", lambda c=c, j=j, pi=pi, hb=hb, s=s: nc.scalar.activation(
                            out=h32[hb][:, j, :], in_=ps[pi][:, j * 128:(j + 1) * 128], func=AF.Identity,
                            scale=ab[:, s, 0, c:c + 1], bias=ab[:, s, 2, c:c + 1]), reads=[("ps", pi), "ab"],
                            writes=[("h32", hb, j)])
                        tk.op("dve", lambda c=c, j=j, pi=pi, s=s, lt=lt: nc.vector.tensor_scalar(
                            out=hT[:, c, lt * 128:(lt + 1) * 128], in0=ps[pi][:, j * 128:(j + 1) * 128],
                            scalar1=ab[:, s, 0, c:c + 1], scalar2=ab[:, s, 2, c:c + 1], op0=ALU.mult, op1=ALU.add),
                            reads=[("ps", pi), "ab"], writes=[("hT", lt)])
                    for j in range(4):
                        if stop < 4:
                            continue
                        c = c4 * 4 + j
                        tk.op("pe", lambda c=c, j=j, hb=hb: nc.tensor.matmul(
                            ps[pl][:, :NE], lhsT=h32[hb][:, j, :], rhs=wr[:, c, :], start=(c == 0),
                            stop=(c == kc - 1)), reads=[("h32", hb, j), "wr"], writes=[("ps", pl)])
                if stop < 5:
                    continue
                tk.op("dve", lambda: nc.vector.tensor_tensor(out=lg[:], in0=ps[pl][:, :NE], in1=brb[:], op=ALU.add),
                      reads=[("ps", pl), "brb"], writes=["lg"])
                tk.op("dve", lambda: nc.vector.max(out=sm[:, 16:24], in_=lg[:]), reads=["lg"], writes=["mx8"])
                tk.op("dve", lambda: nc.vector.tensor_scalar(out=mk[:], in0=lg[:], scalar1=sm[:, 19:20], scalar2=None,
                                                             op0=ALU.is_ge), reads=["lg", "mx8"], writes=["mk"])
                tk.op("dve", lambda: nc.vector.tensor_scalar(out=sm[:, 24:25], in0=sm[:, 16:17], scalar1=-1.0,
                                                             scalar2=None, op0=ALU.mult), reads=["mx8"],
                      writes=["negm"])
                tk.op("act", lambda: nc.scalar.activation(out=ex[:], in_=lg[:], func=AF.Exp, bias=sm[:, 24:25]),
                      reads=["lg", "negm"], writes=["ex"])
                tk.op("dve", lambda: nc.vector.tensor_tensor(out=ex[:], in0=ex[:], in1=mk[:], op=ALU.mult),
                      reads=["ex", "mk"], writes=["ex"])
                tk.op("dve", lambda: nc.vector.tensor_reduce(out=sm[:, 25:26], in_=ex[:], axis=AX.X, op=ALU.add),
                      reads=["ex"], writes=["esum"])
                tk.op("dve", lambda: nc.vector.reciprocal(out=sm[:, 26:27], in_=sm[:, 25:26]), reads=["esum"],
                      writes=["ersum"])
                tk.op("dve", lambda lt=lt: nc.vector.tensor_scalar(out=G[:, lt, :NE], in0=ex[:], scalar1=sm[:, 26:27],
                                                                   scalar2=None, op0=ALU.mult),
                      reads=["ex", "ersum"], writes=[("G", lt)])
                if stop < 6:
                    continue
                tk.op("pe", lambda lt=lt: nc.tensor.transpose(out=ps[6][:, 0:128], in_=G[:, lt, :],
                                                              identity=ident[:]), reads=[("G", lt), "ident"],
                      writes=[("ps", 6)])
                tk.op("act", lambda: nc.scalar.copy(out=GT[:, :], in_=ps[6][:, 0:128]), reads=[("ps", 6)],
                      writes=["GT"])
                for n in range(4):
                    pi = 2 + n
                    tk.op("pe", lambda n=n, pi=pi: nc.tensor.matmul(ps[pi][:, :], lhsT=GT[:, :],
                                                                    rhs=bd[:, n * 512:(n + 1) * 512], start=True,
                                                                    stop=True), reads=["GT", "bd"],
                          writes=[("ps", pi)])
                    tk.op("act", lambda n=n, pi=pi, lt=lt: nc.scalar.copy(out=acc[:, lt, n * 512:(n + 1) * 512],
                                                                         in_=ps[pi][:, :]), reads=[("ps", pi)],
                          writes=[("acc", lt, n)])
            groups = []
            o = 0
            while o < ntok:
                gsz = min(512, ntok - o)
                groups.append((o, gsz))
                o += gsz
            hreads = [("hT", lt) for lt in range(P)]
            for e in range(dbg_ne):
                for j in range(NJ):
                    w = wi % 2
                    wi += 1
                    for half in range(2):
                        c0 = half * FF + j * 128
                        tk.dma("pool", wg[w][:, :, half, :],
                               wgu[e][:, c0:c0 + 128].rearrange("(c p) n -> p c n", p=128),
                               writes=[("wg", w, half)])
                    for (o, gsz) in groups:
                        for half in range(2):
                            pi = half
                            for c in range(kc):
                                tk.op("pe", lambda c=c, w=w, half=half, pi=pi, o=o, gsz=gsz: nc.tensor.matmul(
                                    ps[pi][:, :gsz], lhsT=wg[w][:, c, half, :], rhs=hT[:, c, o:o + gsz],
                                    start=(c == 0), stop=(c == kc - 1)),
                                    reads=hreads + [("wg", w, half)], writes=[("ps", pi)])
                        tk.op("dve", lambda e=e, j=j, gsz=gsz: nc.vector.tensor_scalar(
                            out=t1[:, :gsz], in0=ps[0][:, :gsz], scalar1=bgu[:, e, j:j + 1], scalar2=7.0,
                            op0=ALU.add, op1=ALU.min), reads=[("ps", 0), "bgu"], writes=["t1"])
                        tk.op("act", lambda gsz=gsz: nc.scalar.activation(out=t2[:, :gsz], in_=t1[:, :gsz],
                                                                         func=AF.Sigmoid, scale=1.702),
                              reads=["t1"], writes=["t2"])
                        tk.op("dve", lambda e=e, j=j, gsz=gsz: nc.vector.tensor_scalar(
                            out=t3[:, :gsz], in0=ps[1][:, :gsz], scalar1=bgu[:, e, NJ + j:NJ + j + 1], scalar2=7.0,
                            op0=ALU.add, op1=ALU.min), reads=[("ps", 1), "bgu"], writes=["t3"])
                        tk.op("pool", lambda gsz=gsz: nc.gpsimd.tensor_scalar(
                            out=t3[:, :gsz], in0=t3[:, :gsz], scalar1=-7.0, scalar2=1.0, op0=ALU.max, op1=ALU.add),
                            reads=["t3"], writes=["t3"])
                        tk.op("pool", lambda gsz=gsz: nc.gpsimd.tensor_tensor(
                            out=t1[:, :gsz], in0=t1[:, :gsz], in1=t2[:, :gsz], op=ALU.mult), reads=["t1", "t2"],
                            writes=["t1"])
                        tk.op("dve", lambda j=j, o=o, gsz=gsz: nc.vector.tensor_tensor(
                            out=actT[:, j, o:o + gsz], in0=t1[:, :gsz], in1=t3[:, :gsz], op=ALU.mult),
                            reads=["t1", "t3"], writes=[("actT", j)])
                tk.dma("pool", Wd[:], wdn[e].rearrange("(j p) d -> p j d", p=128), writes=["Wd"])
                areads = [("actT", j) for j in range(NJ)]
                k = 0
                for lt in range(P):
                    for n in range(4):
                        pi = 2 + (k % 4)
                        k += 1
                        for j in range(NJ):
                            tk.op("pe", lambda j=j, lt=lt, n=n, pi=pi: nc.tensor.matmul(
                                ps[pi][:, :], lhsT=actT[:, j, lt * 128:(lt + 1) * 128],
                                rhs=Wd[:, j, n * 512:(n + 1) * 512], start=(j == 0), stop=(j == NJ - 1)),
                                reads=areads + ["Wd"], writes=[("ps", pi)])
                        tk.op("dve", lambda lt=lt, n=n, pi=pi, e=e: nc.vector.scalar_tensor_tensor(
                            out=acc[:, lt, n * 512:(n + 1) * 512], in0=ps[pi][:, :], scalar=G[:, lt, e:e + 1],
                            in1=acc[:, lt, n * 512:(n + 1) * 512], op0=ALU.mult, op1=ALU.add),
                            reads=[("ps", pi), ("G", lt), ("acc", lt, n)], writes=[("acc", lt, n)])
            for lt in range(P):
                t = tile0 + lt
                s = 0 if t < n0 else 1
                b = t % 2
                tk.dma("sp", xt[b][:], x1[t * 128:(t + 1) * 128, :], writes=[("xt", b)])
                tk.op("dve", lambda lt=lt, s=s: nc.vector.tensor_tensor(out=acc[:, lt, :], in0=acc[:, lt, :],
                                                                        in1=mrow[:, s, :], op=ALU.mult),
                      reads=[("acc", lt, n) for n in range(4)] + [("mrow", s)],
                      writes=[("acc", lt, n) for n in range(4)])
                tk.op("pool", lambda lt=lt, b=b: nc.gpsimd.tensor_tensor(out=xt[b][:], in0=xt[b][:],
                                                                        in1=acc[:, lt, :], op=ALU.add),
                      reads=[("acc", lt, n) for n in range(4)] + [("xt", b)], writes=[("xt", b)])
                tk.dma("sp", out[t * 128:(t + 1) * 128, :], xt[b][:], reads=[("xt", b)])
            tile0 += P
        tk.finish()
    return nc


NEL = 4


def build_moe_ep(T, P, nel=NEL):
    nc = bass.Bass("TRN2", target_bir_lowering=False)
    kc = D // 128
    nt = T // 128
    hT_in = nc.dram_tensor("hT_in", [D, T], BF16, kind="ExternalInput").ap()
    lg_in = nc.dram_tensor("lg_in", [T, NE], F32, kind="ExternalInput").ap()
    wgu = nc.dram_tensor("wgu", [nel, D, 2 * FF], F32, kind="ExternalInput").ap()
    bguT = nc.dram_tensor("bguT", [128, nel, 2 * NJ], F32, kind="ExternalInput").ap()
    wdn = nc.dram_tensor("wdn", [nel, FF, D], F32, kind="ExternalInput").ap()
    bdn = nc.dram_tensor("bdn", [nel, D], F32, kind="ExternalInput").ap()
    out = nc.dram_tensor("out", [T, D], BF16, kind="ExternalOutput").ap()
    with ExitStack() as es:
        tk = TK(nc, es)
        sb = lambda name, shape, dt: es.enter_context(nc.sbuf_tensor(name, shape, dt))
        ps = [es.enter_context(nc.psum_tensor("ps%d" % i, [128, 512], F32)) for i in range(8)]
        ident = sb("ident", [128, 128], F32)
        tk.op("pool", lambda: nc.gpsimd.memset(ident[:], 0.0), writes=["ident"])
        tk.op("pool", lambda: nc.gpsimd.affine_select(out=ident[:], in_=ident[:], pattern=[[-1, 128]],
                                                      compare_op=ALU.not_equal, fill=1.0, base=0,
                                                      channel_multiplier=1), reads=["ident"], writes=["ident"])
        negbig = sb("negbig", [128, NE], F32)
        tk.op("pool", lambda: nc.gpsimd.memset(negbig[:], -1.0e30), writes=["negbig"])
        onesr = sb("onesr", [128, NE], F32)
        tk.op("pool", lambda: nc.gpsimd.memset(onesr[:], 1.0), writes=["onesr"])
        zer = sb("zer", [128, NE], F32)
        tk.op("pool", lambda: nc.gpsimd.memset(zer[:], 0.0), writes=["zer"])
        sev = sb("sev", [128, 512], F32)
        tk.op("pool", lambda: nc.gpsimd.memset(sev[:], 7.0), writes=["sev"])
        bgu = sb("bgu", [128, nel, 2 * NJ], F32)
        tk.dma("sp", bgu[:], bguT, writes=["bgu"])
        bd = sb("bd", [128, D], BF16)
        tk.op("pool", lambda: nc.gpsimd.memset(bd[:], 0.0), writes=["bd"])
        tk.dma("pool", bd[:nel, :], bdn, reads=["bd"], writes=["bd"])
        hT = sb("hT", [128, kc, P * 128], BF16)
        acc = sb("acc", [128, P, D], F32)
        actT = sb("actT", [128, NJ, P * 128], BF16)
        Wd = sb("Wd", [128, NJ, D], BF16)
        wg = [sb("wg%d" % i, [128, kc, 2, 128], BF16) for i in range(2)]
        t1 = sb("t1", [128, 512], F32)
        t2 = sb("t2", [128, 512], F32)
        t3 = sb("t3", [128, 512], F32)
        G = sb("G", [128, P, 128], F32)
        tk.op("pool", lambda: nc.gpsimd.memset(G[:], 0.0), writes=[("G", i) for i in range(P)])
        GT = sb("GT", [128, 128], BF16)
        sm = sb("sm", [128, 16], F32)
        lg = sb("lg", [128, NE], F32)
        wk = sb("wk", [128, NE], F32)
        eq = sb("eq", [128, NE], F32)
        ex = sb("ex", [128, NE], F32)
        wi = 0
        tile0 = 0
        while tile0 < nt:
            Pp = min(P, nt - tile0)
            ntok = Pp * 128
            tok0 = tile0 * 128
            for c in range(kc):
                tk.dma("pool", hT[:, c, :ntok], hT_in[c * 128:(c + 1) * 128, tok0:tok0 + ntok], writes=[("hT", c)])
            for lt in range(Pp):
                t = tile0 + lt
                tk.dma("sp", lg[:], lg_in[t * 128:(t + 1) * 128, :], writes=["lg"])
                tk.op("dve", lambda: nc.vector.tensor_copy(out=wk[:], in_=lg[:]), reads=["lg"], writes=["wk"])
                for r in range(4):
                    tk.op("dve", lambda r=r: nc.vector.tensor_reduce(out=sm[:, r:r + 1], in_=wk[:], axis=AX.X,
                                                                     op=ALU.max), reads=["wk"], writes=[("m", r)])
                    if r < 3:
                        tk.op("dve", lambda r=r: nc.vector.scalar_tensor_tensor(
                            out=eq[:], in0=wk[:], scalar=sm[:, r:r + 1], in1=negbig[:], op0=ALU.is_equal,
                            op1=ALU.mult), reads=["wk", ("m", r), "negbig"], writes=["eq"])
                        tk.op("dve", lambda: nc.vector.tensor_tensor(out=wk[:], in0=wk[:], in1=eq[:], op=ALU.add),
                              reads=["wk", "eq"], writes=["wk"])
                tk.op("dve", lambda: nc.vector.scalar_tensor_tensor(
                    out=eq[:], in0=lg[:], scalar=sm[:, 3:4], in1=onesr[:], op0=ALU.is_ge, op1=ALU.mult),
                    reads=["lg", ("m", 3), "onesr"], writes=["eq"])
                tk.op("dve", lambda: nc.vector.scalar_tensor_tensor(
                    out=ex[:], in0=lg[:], scalar=sm[:, 0:1], in1=zer[:], op0=ALU.subtract, op1=ALU.add),
                    reads=["lg", ("m", 0), "zer"], writes=["ex"])
                tk.op("act", lambda: nc.scalar.activation(out=ex[:], in_=ex[:], func=AF.Exp), reads=["ex"],
                      writes=["ex"])
                tk.op("dve", lambda: nc.vector.tensor_tensor(out=ex[:], in0=ex[:], in1=eq[:], op=ALU.mult),
                      reads=["ex", "eq"], writes=["ex"])
                tk.op("dve", lambda: nc.vector.tensor_reduce(out=sm[:, 8:9], in_=ex[:], axis=AX.X, op=ALU.add),
                      reads=["ex"], writes=["esum"])
                tk.op("dve", lambda: nc.vector.reciprocal(out=sm[:, 9:10], in_=sm[:, 8:9]), reads=["esum"],
                      writes=["ersum"])
                tk.op("dve", lambda lt=lt: nc.vector.scalar_tensor_tensor(
                    out=G[:, lt, :NE], in0=ex[:], scalar=sm[:, 9:10], in1=onesr[:], op0=ALU.mult, op1=ALU.mult),
                    reads=["ex", "ersum", "onesr"], writes=[("G", lt)])
                tk.op("pe", lambda lt=lt: nc.tensor.transpose(out=ps[6][:, 0:128], in_=G[:, lt, :],
                                                              identity=ident[:]), reads=[("G", lt), "ident"],
                      writes=[("ps", 6)])
                tk.op("act", lambda: nc.scalar.copy(out=GT[:, :], in_=ps[6][:, 0:128]), reads=[("ps", 6)],
                      writes=["GT"])
                for n in range(4):
                    pi = 2 + n
                    tk.op("pe", lambda n=n, pi=pi: nc.tensor.matmul(ps[pi][:, :], lhsT=GT[:, :],
                                                                    rhs=bd[:, n * 512:(n + 1) * 512], start=True,
                                                                    stop=True), reads=["GT", "bd"],
                          writes=[("ps", pi)])
                    tk.op("act", lambda n=n, pi=pi, lt=lt: nc.scalar.copy(out=acc[:, lt, n * 512:(n + 1) * 512],
                                                                         in_=ps[pi][:, :]), reads=[("ps", pi)],
                          writes=[("acc", lt, n)])
            groups = []
            o = 0
            while o < ntok:
                gsz = min(512, ntok - o)
                groups.append((o, gsz))
                o += gsz
            hreads = [("hT", c) for c in range(kc)]
            for e in range(nel):
                for j in range(NJ):
                    w = wi % 2
                    wi += 1
                    for half in range(2):
                        c0 = half * FF + j * 128
                        tk.dma("pool", wg[w][:, :, half, :],
                               wgu[e][:, c0:c0 + 128].rearrange("(c p) n -> p c n", p=128),
                               writes=[("wg", w, half)])
                    for (o, gsz) in groups:
                        for half in range(2):
                            pi = half
                            for c in range(kc):
                                tk.op("pe", lambda c=c, w=w, half=half, pi=pi, o=o, gsz=gsz: nc.tensor.matmul(
                                    ps[pi][:, :gsz], lhsT=wg[w][:, c, half, :], rhs=hT[:, c, o:o + gsz],
                                    start=(c == 0), stop=(c == kc - 1)),
                                    reads=hreads + [("wg", w, half)], writes=[("ps", pi)])
                        tk.op("dve", lambda e=e, j=j, gsz=gsz: nc.vector.scalar_tensor_tensor(
                            out=t1[:, :gsz], in0=ps[0][:, :gsz], scalar=bgu[:, e, j:j + 1], in1=sev[:, :gsz],
                            op0=ALU.add, op1=ALU.min), reads=[("ps", 0), "bgu", "sev"], writes=["t1"])
                        tk.op("act", lambda gsz=gsz: nc.scalar.activation(out=t2[:, :gsz], in_=t1[:, :gsz],
                                                                         func=AF.Sigmoid, scale=1.702),
                              reads=["t1"], writes=["t2"])
                        tk.op("dve", lambda e=e, j=j, gsz=gsz: nc.vector.scalar_tensor_tensor(
                            out=t3[:, :gsz], in0=ps[1][:, :gsz], scalar=bgu[:, e, NJ + j:NJ + j + 1],
                            in1=sev[:, :gsz], op0=ALU.add, op1=ALU.min), reads=[("ps", 1), "bgu", "sev"],
                            writes=["t3"])
                        tk.op("dve", lambda gsz=gsz: nc.vector.tensor_scalar(
                            out=t3[:, :gsz], in0=t3[:, :gsz], scalar1=-7.0, scalar2=1.0, op0=ALU.max, op1=ALU.add),
                            reads=["t3"], writes=["t3"])
                        tk.op("pool", lambda gsz=gsz: nc.gpsimd.tensor_tensor(
                            out=t1[:, :gsz], in0=t1[:, :gsz], in1=t2[:, :gsz], op=ALU.mult), reads=["t1", "t2"],
                            writes=["t1"])
                        tk.op("dve", lambda j=j, o=o, gsz=gsz: nc.vector.tensor_tensor(
                            out=actT[:, j, o:o + gsz], in0=t1[:, :gsz], in1=t3[:, :gsz], op=ALU.mult),
                            reads=["t1", "t3"], writes=[("actT", j)])
                tk.dma("pool", Wd[:], wdn[e].rearrange("(j p) d -> p j d", p=128), writes=["Wd"])
                areads = [("actT", j) for j in range(NJ)]
                k = 0
                for lt in range(Pp):
                    for n in range(4):
                        pi = 2 + (k % 4)
                        k += 1
                        for j in range(NJ):
                            tk.op("pe", lambda j=j, lt=lt, n=n, pi=pi: nc.tensor.matmul(
                                ps[pi][:, :], lhsT=actT[:, j, lt * 128:(lt + 1) * 128],
                                rhs=Wd[:, j, n * 512:(n + 1) * 512], start=(j == 0), stop=(j == NJ - 1)),
                                reads=areads + ["Wd"], writes=[("ps", pi)])
                        tk.op("dve", lambda lt=lt, n=n, pi=pi, e=e: nc.vector.scalar_tensor_tensor(
                            out=acc[:, lt, n * 512:(n + 1) * 512], in0=ps[pi][:, :], scalar=G[:, lt, e:e + 1],
                            in1=acc[:, lt, n * 512:(n + 1) * 512], op0=ALU.mult, op1=ALU.add),
                            reads=[("ps", pi), ("G", lt), ("acc", lt, n)], writes=[("acc", lt, n)])
            for lt in range(Pp):
                t = tile0 + lt
                tk.dma("pool", out[t * 128:(t + 1) * 128, :], acc[:, lt, :],
                       reads=[("acc", lt, n) for n in range(4)])
            tile0 += Pp
        tk.finish()
    return nc


def build_combine(T, n0, nparts=8):
    nc = bass.Bass("TRN2", target_bir_lowering=False)
    nt = T // 128
    parts = nc.dram_tensor("parts", [nparts, T, D], BF16, kind="ExternalInput").ap()
    x1 = nc.dram_tensor("x1", [T, D], F32, kind="ExternalInput").ap()
    modrow = nc.dram_tensor("modrow", [2, D], F32, kind="ExternalInput").ap()
    out = nc.dram_tensor("out", [T, D], F32, kind="ExternalOutput").ap()
    with ExitStack() as es:
        tk = TK(nc, es)
        sb = lambda name, shape, dt: es.enter_context(nc.sbuf_tensor(name, shape, dt))
        mrow = sb("mrow", [128, 2, D], F32)
        for s in range(2):
            tk.dma("pool", mrow[:, s, :], modrow[s:s + 1, :].partition_broadcast(128), writes=[("mrow", s)])
        pb = [sb("pb%d" % i, [128, D], F32) for i in range(4)]
        ac = [sb("ac%d" % i, [128, D], F32) for i in range(2)]
        xb = [sb("xb%d" % i, [128, D], F32) for i in range(2)]
        k = 0
        for t in range(nt):
            b = t % 2
            s = 0 if t < n0 else 1
            tk.dma("sp", xb[b][:], x1[t * 128:(t + 1) * 128, :], writes=[("xb", b)])
            tk.dma("pool", ac[b][:], parts[0, t * 128:(t + 1) * 128, :], writes=[("ac", b)])
            for p in range(1, nparts):
                pi = k % 4
                k += 1
                tk.dma("pool", pb[pi][:], parts[p, t * 128:(t + 1) * 128, :], writes=[("pb", pi)])
                eng = "dve" if p % 2 else "pool"
                E = nc.vector if p % 2 else nc.gpsimd
                tk.op(eng, lambda E=E, b=b, pi=pi: E.tensor_tensor(out=ac[b][:], in0=ac[b][:], in1=pb[pi][:],
                                                                  op=ALU.add),
                      reads=[("ac", b), ("pb", pi)], writes=[("ac", b)])
            tk.op("dve", lambda b=b, s=s: nc.vector.tensor_tensor(out=ac[b][:], in0=ac[b][:], in1=mrow[:, s, :],
                                                                  op=ALU.mult),
                  reads=[("ac", b), ("mrow", s)], writes=[("ac", b)])
            tk.op("pool", lambda b=b: nc.gpsimd.tensor_tensor(out=ac[b][:], in0=ac[b][:], in1=xb[b][:], op=ALU.add),
                  reads=[("ac", b), ("xb", b)], writes=[("ac", b)])
            tk.dma("sp", out[t * 128:(t + 1) * 128, :], ac[b][:], reads=[("ac", b)])
        tk.finish()
    return nc


SSD_HL = 32
SSD_GL = 4
LGROUPS = [(0, 2)] + [(2 + 4 * i, 4) for i in range(8)]


def _ssd_iters(a, n):
    its = []
    for st in range(NTT):
        if a == 0:
            if st < 2:
                its.append((st, 0, st))
                its.append((st, 1, 4 + st))
        else:
            if st < a:
                its.append((st, 0, None))
                if st < 2:
                    its.append((st, 1, None))
            elif st < a + n:
                its.append((st, 0, st - a))
                its.append((st, 1, 4 + st - a))
            else:
                its.append((st, 1, None))
    return its


def build_ssd(nheads=SSD_HL, ngroups=SSD_GL, stop=9, var=''):
    nc = bass.Bass("TRN2", target_bir_lowering=False)
    hpg = nheads // ngroups
    xT_in = nc.dram_tensor("xT", [nheads, 64, NTOK], F32, kind="ExternalInput").ap()
    bc_in = nc.dram_tensor("bcT", [2 * ngroups, 128, NTOK], F32, kind="ExternalInput").ap()
    cwx = nc.dram_tensor("cwx", [64, nheads, 6], F32, kind="ExternalInput").ap()
    cwb = nc.dram_tensor("cwb", [128, 2 * ngroups, 6], F32, kind="ExternalInput").ap()
    dt_in = nc.dram_tensor("dtT", [64, NTOK], F32, kind="ExternalInput").ap()
    dpar = nc.dram_tensor("dpar", [64, 2], F32, kind="ExternalInput").ap()
    dcol_in = nc.dram_tensor("dcol", [64, nheads], F32, kind="ExternalInput").ap()
    masks = nc.dram_tensor("masks", [8, 128, 512], F32, kind="ExternalInput").ap()
    yT = nc.dram_tensor("yT", [nheads * 64, NTOK], F32, kind="ExternalOutput").ap()
    Qd = nc.dram_tensor("Qd", [64, NTOK], F32, kind="ExternalOutput").ap()
    with ExitStack() as es:
        tk = TK(nc, es)
        sb = lambda name, shape, dt: es.enter_context(nc.sbuf_tensor(name, shape, dt))
        ps = [es.enter_context(nc.psum_tensor("ps%d" % i, [128, 512], F32)) for i in range(8)]
        ident = sb("ident", [128, 128], F32)
        tk.op("pool", lambda: nc.gpsimd.memset(ident[:], 0.0), writes=["ident"])
        tk.op("pool", lambda: nc.gpsimd.affine_select(out=ident[:], in_=ident[:], pattern=[[-1, 128]],
                                                      compare_op=ALU.not_equal, fill=1.0, base=0,
                                                      channel_multiplier=1), reads=["ident"], writes=["ident"])
        onec = sb("onec", [128, 1], F32)
        tk.op("pool", lambda: nc.gpsimd.memset(onec[:], 1.0), writes=["onec"])
        W = [sb("W%d" % i, [128, NTOK], F32) for i in range(4)]
        tk.op("pool", lambda: nc.gpsimd.memset(W[3][64:128, :], 0.0), writes=["W3hi"])
        mk = sb("mk8", [128, 8, 512], BF16)
        for j in range(8):
            tk.dma("pool", mk[:, j, :], masks[j], writes=[("mk", j)])
        cx = sb("cx", [64, nheads, 6], F32)
        tk.dma("sp", cx[:], cwx, writes=["cx"])
        cb = sb("cb", [128, 2 * ngroups, 6], F32)
        tk.dma("sp", cb[:], cwb, writes=["cb"])
        dp = sb("dp", [64, 4], F32)
        tk.dma("sp", dp[:, 0:2], dpar, writes=["dp"])
        dcol = sb("dcol_sb", [64, nheads], F32)
        tk.dma("sp", dcol[:], dcol_in, writes=["dcol"])
        Qtok = sb("Qtok", [128, NTT, 64], F32)
        dtok = sb("dtok", [128, NTT, 64], F32)
        BT = sb("BT", [128, NTOK], BF16)
        CT = sb("CT", [128, NTOK], BF16)
        Xtok = sb("Xtok", [128, NTT, 64], BF16)
        xdt = sb("xdt", [128, NTT, 2, 64], BF16)
        Qrow = [sb("Qrow%d" % i, [128, 512], F32) for i in range(4)]
        Ef = [sb("Ef%d" % i, [128, 512], F32) for i in range(3)]
        Mt = [sb("Mt%d" % i, [128, 512], BF16) for i in range(3)]
        yo = [sb("yo%d" % i, [64, 512], F32) for i in range(2)]

        def ones_b(p, n):
            return onec[:p, 0:1].broadcast_to([p, n])

        dt, a_, Q, tmp = W[0], W[1], W[2], W[3]
        tk.dma("sp", dt[:64, :], dt_in, writes=["W0"])
        tk.op("dve", lambda: nc.vector.scalar_tensor_tensor(out=dt[:64, :], in0=dt[:64, :], scalar=dp[:, 0:1],
                                                            in1=ones_b(64, NTOK), op0=ALU.add, op1=ALU.mult),
              reads=["W0", "dp", "onec"], writes=["W0"])
        tk.op("act", lambda: nc.scalar.activation(out=dt[:64, :], in_=dt[:64, :], func=AF.Exp), reads=["W0"],
              writes=["W0"])
        tk.op("act", lambda: nc.scalar.activation(out=dt[:64, :], in_=dt[:64, :], func=AF.Ln, bias=1.0),
              reads=["W0"], writes=["W0"])
        tk.op("act", lambda: nc.scalar.activation(out=dp[:, 2:3], in_=dp[:, 1:2], func=AF.Exp), reads=["dp"],
              writes=["dpA"])
        tk.op("dve", lambda: nc.vector.tensor_scalar(out=dp[:, 2:3], in0=dp[:, 2:3], scalar1=-1.0, scalar2=None,
                                                     op0=ALU.mult), reads=["dpA"], writes=["dpA"])
        tk.op("dve", lambda: nc.vector.scalar_tensor_tensor(out=a_[:64, :], in0=dt[:64, :], scalar=dp[:, 2:3],
                                                            in1=ones_b(64, NTOK), op0=ALU.mult, op1=ALU.mult),
              reads=["W0", "dpA", "onec"], writes=["W1"])
        if stop < 1:
            tk.dma("sp", Qd[:, :], a_[:64, :], reads=["W1"], writes=["Qd"])
            tk.finish()
            return nc
        tk.op("dve", lambda: nc.vector.tensor_tensor_scan(out=Q[0:32, :], data0=ones_b(32, NTOK), data1=a_[0:32, :],
                                                          initial=0.0, op0=ALU.mult, op1=ALU.add),
              reads=["W1", "onec"], writes=["W2"])
        tk.op("dve", lambda: nc.vector.tensor_tensor_scan(out=tmp[32:64, 0:256],
                                                          data0=onec[32:64, 0:1].broadcast_to([32, 256]),
                                                          data1=a_[32:64, 0:256], initial=0.0, op0=ALU.mult,
                                                          op1=ALU.add), reads=["W1", "onec"], writes=["W3"])
        tk.op("dve", lambda: nc.vector.tensor_tensor_scan(out=tmp[32:64, 256:NTOK],
                                                          data0=onec[32:64, 0:1].broadcast_to([32, NTOK - 256]),
                                                          data1=a_[32:64, 256:NTOK], initial=0.0, op0=ALU.mult,
                                                          op1=ALU.add), reads=["W1", "onec"], writes=["W3"])
        tk.op("dve", lambda: nc.vector.tensor_tensor(out=Q[32:64, :], in0=a_[32:64, :], in1=tmp[32:64, :],
                                                     op=ALU.subtract), reads=["W1", "W3"], writes=["W2"])
        tk.op("dve", lambda: nc.vector.scalar_tensor_tensor(
            out=Q[32:64, 256:NTOK], in0=Q[32:64, 256:NTOK], scalar=tmp[32:64, NTOK - 1:NTOK],
            in1=onec[32:64, 0:1].broadcast_to([32, NTOK - 256]), op0=ALU.add, op1=ALU.mult),
            reads=["W2", "W3", "onec"], writes=["W2"])
        tk.dma("sp", Qd[:, :], Q[:64, :], reads=["W2"], writes=["Qd"])
        if stop < 2:
            tk.finish()
            return nc
        if 'c' in var:
            tk.op("pool", lambda: nc.gpsimd.memset(Q[64:128, :], 0.0), writes=["W2hi"])
        else:
            tk.dma("sp", Q[64:128, :], dt[:64, :], reads=["W0"], writes=["W2hi"])
        for t in range(1 if 'a' in var else NTT):
            pi = 6 + (t % 2)
            tk.op("pe", lambda t=t, pi=pi: nc.tensor.transpose(out=ps[pi][:, 0:128], in_=Q[:, t * 128:(t + 1) * 128],
                                                               identity=ident[:]),
                  reads=["W2", "W2hi", "ident"], writes=[("ps", pi)])
            if True:
                tk.op("dve", lambda t=t, pi=pi: nc.vector.tensor_copy(out=Qtok[:, t, :], in_=ps[pi][:, 0:64]),
                      reads=[("ps", pi)], writes=["Qtok"])
            else:
                tk.op("act", lambda t=t, pi=pi: nc.scalar.copy(out=Qtok[:, t, :], in_=ps[pi][:, 0:64]),
                      reads=[("ps", pi)], writes=["Qtok"])
            tk.op("dve", lambda t=t, pi=pi: nc.vector.tensor_copy(out=dtok[:, t, :], in_=ps[pi][:, 64:128]),
                  reads=[("ps", pi)], writes=["dtok"])
        if stop < 3:
            tk.finish()
            return nc
        SEGS = [(0, 256), (256, NTOK)]

        def conv_silu(src, P, wts, ci, dst, dst_key, srckey):
            o = W[2]
            first = True
            for (s0, s1) in SEGS:
                tk.op("dve", lambda s0=s0, s1=s1: nc.vector.scalar_tensor_tensor(
                    out=o[:P, s0:s1], in0=src[:P, s0:s1], scalar=wts[:P, ci, 2:3],
                    in1=wts[:P, ci, 5:6].broadcast_to([P, s1 - s0]), op0=ALU.mult, op1=ALU.add),
                    reads=[srckey, "cx", "cb"], writes=["W2", "W2hi"])
                for k in (0, 1, 3, 4):
                    sh = k - 2
                    lo = max(s0, s0 - sh)
                    hi = min(s1, s1 - sh)
                    tk.op("dve", lambda lo=lo, hi=hi, sh=sh, k=k: nc.vector.scalar_tensor_tensor(
                        out=o[:P, lo:hi], in0=src[:P, lo + sh:hi + sh], scalar=wts[:P, ci, k:k + 1],
                        in1=o[:P, lo:hi], op0=ALU.mult, op1=ALU.add), reads=[srckey, "W2", "cx", "cb"],
                        writes=["W2"])
            tk.op("act", lambda: nc.scalar.activation(out=dst, in_=o[:P, :], func=AF.Silu), reads=["W2"],
                  writes=[dst_key])

        qk = 0
        it3 = 0
        lgk = 0
        for g in range(ngroups):
            tk.dma("sp", W[0][:, :], bc_in[g], reads=["W0"], writes=["W0"])
            conv_silu(W[0], 128, cb, g, BT[:, :], "BT", "W0")
            tk.dma("sp", W[1][:, :], bc_in[ngroups + g], reads=["W1"], writes=["W1"])
            conv_silu(W[1], 128, cb, ngroups + g, CT[:, :], "CT", "W1")
            for hh in range(hpg):
                h = g * hpg + hh
                wb = h % 2
                src = W[wb]
                skey = "W%d" % wb
                tk.dma("sp", src[:64, :], xT_in[h], reads=[skey], writes=[skey])
                xc = W[3]
                conv_silu(src, 64, cx, h, xc[:64, :], "W3", skey)
                for t in range(NTT):
                    pi = 4 + (t % 2)
                    tk.op("pe", lambda t=t, pi=pi: nc.tensor.transpose(out=ps[pi][:, 0:128],
                                                                       in_=xc[:, t * 128:(t + 1) * 128],
                                                                       identity=ident[:]),
                          reads=["W3", "W3hi", "ident"], writes=[("ps", pi)])
                    for d_ in range(2):
                        E = nc.vector
                        tk.op("dve", lambda t=t, pi=pi, d_=d_, h=h: nc.vector.tensor_tensor(
                            out=xdt[:, t, d_, :], in0=ps[pi][:, 0:64],
                            in1=dtok[:, t, d_ * 32 + h:d_ * 32 + h + 1].broadcast_to([128, 64]), op=ALU.mult),
                            reads=[("ps", pi), "dtok"], writes=["xdt"])
                for (a, n) in LGROUPS:
                    nl = n * 128
                    l0 = a * 128
                    pacc = 2 + (lgk % 2)
                    lgk += 1
                    qr = []
                    for d_ in range(2):
                        qi = qk % 4
                        qk += 1
                        tk.dma("pool", Qrow[qi][:, :nl], Qd[d_ * 32 + h:d_ * 32 + h + 1, l0:l0 + nl]
                               .partition_broadcast(128), reads=["Qd"], writes=[("Qrow", qi)])
                        qr.append(qi)
                    its = _ssd_iters(a, n)
                    last_st = None
                    for ii, (st, d_, mi) in enumerate(its):
                        if st != last_st:
                            pg = it3 % 2
                            it3 += 1
                            last_st = st
                            tk.op("pe", lambda st=st, pg=pg, l0=l0, nl=nl: nc.tensor.matmul(
                                ps[pg][:, :nl], lhsT=BT[:, st * 128:(st + 1) * 128], rhs=CT[:, l0:l0 + nl],
                                start=True, stop=True), reads=["BT", "CT"], writes=[("ps", pg)])
                        e = ii % 3
                        qi = qr[d_]
                        col = d_ * 32 + h
                        tk.op("pool", lambda e=e, qi=qi, st=st, col=col, nl=nl: nc.gpsimd.tensor_tensor(
                            out=Ef[e][:, :nl], in0=Qrow[qi][:, :nl],
                            in1=Qtok[:, st, col:col + 1].broadcast_to([128, nl]), op=ALU.subtract),
                            reads=[("Qrow", qi), "Qtok"], writes=[("Ef", e)])
                        if mi is not None:
                            tk.op("pool", lambda e=e, mi=mi, nl=nl: nc.gpsimd.tensor_tensor(
                                out=Ef[e][:, :nl], in0=Ef[e][:, :nl], in1=mk[:, mi, :nl], op=ALU.add),
                                reads=[("Ef", e), ("mk", mi)], writes=[("Ef", e)])
                        tk.op("act", lambda e=e, nl=nl: nc.scalar.activation(out=Ef[e][:, :nl], in_=Ef[e][:, :nl],
                                                                            func=AF.Exp), reads=[("Ef", e)],
                              writes=[("Ef", e)])
                        tk.op("dve", lambda e=e, pg=pg, nl=nl: nc.vector.tensor_tensor(
                            out=Mt[e][:, :nl], in0=ps[pg][:, :nl], in1=Ef[e][:, :nl], op=ALU.mult),
                            reads=[("ps", pg), ("Ef", e)], writes=[("Mt", e)])
                        tk.op("pe", lambda e=e, st=st, d_=d_, pacc=pacc, nl=nl, ii=ii, its=its: nc.tensor.matmul(
                            ps[pacc][:64, :nl], lhsT=xdt[:, st, d_, :], rhs=Mt[e][:, :nl], start=(ii == 0),
                            stop=(ii == len(its) - 1)), reads=["xdt", ("Mt", e)], writes=[("ps", pacc)])
                    yb = lgk % 2
                    tk.op("dve", lambda pacc=pacc, yb=yb, h=h, l0=l0, nl=nl: nc.vector.scalar_tensor_tensor(
                        out=yo[yb][:, :nl], in0=xc[:64, l0:l0 + nl], scalar=dcol[:, h:h + 1],
                        in1=ps[pacc][:64, :nl], op0=ALU.mult, op1=ALU.add),
                        reads=["W3", "dcol", ("ps", pacc)], writes=[("yo", yb)])
                    tk.dma("sp", yT[h * 64:(h + 1) * 64, l0:l0 + nl], yo[yb][:, :nl], reads=[("yo", yb)])
        tk.finish()
    return nc


def build_ssdfin(T, n0, NB=256, GT=5):
    nc = bass.Bass("TRN2", target_bir_lowering=False)
    K, N = 4096, D
    nt, kc, nb = T // 128, K // 128, N // NB
    y_in = nc.dram_tensor("y", [T, K], F32, kind="ExternalInput").ap()
    z_in = nc.dram_tensor("z", [T, K], F32, kind="ExternalInput").ap()
    gn = nc.dram_tensor("gn", [1, K], F32, kind="ExternalInput").ap()
    xres = nc.dram_tensor("xres", [T, N], F32, kind="ExternalInput").ap()
    modrow = nc.dram_tensor("modrow", [2, N], F32, kind="ExternalInput").ap()
    w = nc.dram_tensor("w", [K, N], F32, kind="ExternalInput").ap()
    out = nc.dram_tensor("out", [T, N], F32, kind="ExternalOutput").ap()
    with ExitStack() as es:
        tk = TK(nc, es)
        sb = lambda name, shape, dt: es.enter_context(nc.sbuf_tensor(name, shape, dt))
        ps = [es.enter_context(nc.psum_tensor("ps%d" % i, [128, 512], F32)) for i in range(8)]
        ident = sb("ident", [128, 128], F32)
        tk.op("pool", lambda: nc.gpsimd.memset(ident[:], 0.0), writes=["ident"])
        tk.op("pool", lambda: nc.gpsimd.affine_select(out=ident[:], in_=ident[:], pattern=[[-1, 128]],
                                                      compare_op=ALU.not_equal, fill=1.0, base=0,
                                                      channel_multiplier=1), reads=["ident"], writes=["ident"])
        hT = sb("hT", [128, kc, GT * 128], BF16)
        yt = sb("yt", [128, K], F32)
        zt = sb("zt", [128, K], F32)
        gnr = sb("gnr", [128, K], F32)
        tk.dma("pool", gnr[:], gn[0:1, :].partition_broadcast(128), writes=["gnr"])
        mrow = sb("mrow", [128, 2, N], F32)
        for s in range(2):
            tk.dma("pool", mrow[:, s, :], modrow[s:s + 1, :].partition_broadcast(128), writes=[("mrow", s)])
        st = sb("st", [128, 3, 8], F32)
        wbuf = [sb("wb%d" % i, [128, kc, NB], BF16) for i in range(2)]
        ob = [sb("ob%d" % i, [128, NB], F32) for i in range(3)]
        xr = [sb("xr%d" % i, [128, NB], F32) for i in range(3)]
        g3 = lambda t_: t_[:, :].rearrange("p (g d) -> p g d", g=8)
        k = 0
        for g0 in range(0, nt, GT):
            g1 = min(nt, g0 + GT)
            for t in range(g0, g1):
                tk.dma("sp", yt[:], y_in[t * 128:(t + 1) * 128, :], writes=["yt"])
                tk.dma("sp", zt[:], z_in[t * 128:(t + 1) * 128, :], writes=["zt"])
                tk.op("act", lambda: nc.scalar.activation(out=zt[:], in_=zt[:], func=AF.Silu), reads=["zt"], writes=["zt"])
                tk.op("dve", lambda: nc.vector.tensor_tensor(out=yt[:], in0=yt[:], in1=zt[:], op=ALU.mult),
                      reads=["yt", "zt"], writes=["yt"])
                tk.op("act", lambda: nc.scalar.activation(out=zt[:], in_=yt[:], func=AF.Square), reads=["yt", "zt"],
                      writes=["zt"])
                tk.op("dve", lambda: nc.vector.tensor_reduce(out=st[:, 0, :], in_=g3(zt), axis=AX.X, op=ALU.add),
                      reads=["zt"], writes=["st0"])
                tk.op("act", lambda: nc.scalar.activation(out=st[:, 1, :], in_=st[:, 0, :], func=AF.Sqrt,
                                                          scale=1.0 / 512, bias=EPS), reads=["st0"], writes=["st1"])
                tk.op("dve", lambda: nc.vector.reciprocal(out=st[:, 2, :], in_=st[:, 1, :]), reads=["st1"],
                      writes=["st2"])
                tk.op("dve", lambda: nc.vector.tensor_tensor(out=g3(zt), in0=g3(yt),
                                                             in1=st[:, 2, :].unsqueeze(2).broadcast_to([128, 8, 512]),
                                                             op=ALU.mult), reads=["yt", "st2", "zt"], writes=["zt"])
                tk.op("pool", lambda: nc.gpsimd.tensor_tensor(out=zt[:], in0=zt[:], in1=gnr[:], op=ALU.mult),
                      reads=["zt", "gnr"], writes=["zt"])
                for c4 in range(kc // 4):
                    pi = c4 % 2
                    for j in range(4):
                        c = c4 * 4 + j
                        tk.op("pe", lambda c=c, j=j, pi=pi: nc.tensor.transpose(
                            out=ps[pi][:, j * 128:(j + 1) * 128], in_=zt[:, c * 128:(c + 1) * 128],
                            identity=ident[:]), reads=["zt", "ident"], writes=[("ps", pi)])
                    dst = hT[:, c4 * 4:(c4 + 1) * 4, (t - g0) * 128:(t - g0 + 1) * 128]
                    src = ps[pi][:].rearrange("p (j q) -> p j q", j=4)
                    if c4 % 2 == 0:
                        tk.op("act", lambda dst=dst, src=src: nc.scalar.copy(out=dst, in_=src),
                              reads=[("ps", pi)], writes=[("hTt", t, c4)])
                    else:
                        tk.op("dve", lambda dst=dst, src=src: nc.vector.tensor_copy(out=dst, in_=src),
                              reads=[("ps", pi)], writes=[("hTt", t, c4)])
            for n in range(nb):
                n0c = n * NB
                wb = n % 2
                tk.dma("pool", wbuf[wb][:, :, :], w[:, n0c:n0c + NB].rearrange("(c p) n -> p c n", p=128),
                       writes=[("wb", wb)])
                for t in range(g0, g1):
                    pi = 2 + (k % 4)
                    o = k % 3
                    k += 1
                    rd = [("hTt", t, c4) for c4 in range(kc // 4)]
                    for c in range(kc):
                        tk.op("pe", lambda c=c, t=t, pi=pi, wb=wb: nc.tensor.matmul(
                            ps[pi][:, :NB], lhsT=hT[:, c, (t - g0) * 128:(t - g0 + 1) * 128], rhs=wbuf[wb][:, c, :],
                            start=(c == 0), stop=(c == kc - 1)), reads=rd + [("wb", wb)], writes=[("ps", pi)])
                    s = 0 if t < n0 else 1
                    tk.dma("sp", xr[o][:, :], xres[t * 128:(t + 1) * 128, n0c:n0c + NB], writes=[("xr", o)])
                    tk.op("dve", lambda pi=pi, o=o, s=s, n0c=n0c: nc.vector.tensor_tensor(
                        out=ob[o][:, :], in0=ps[pi][:, :NB], in1=mrow[:, s, n0c:n0c + NB], op=ALU.mult),
                        reads=[("ps", pi), ("mrow", s)], writes=[("ob", o)])
                    tk.op("pool", lambda o=o: nc.gpsimd.tensor_tensor(out=ob[o][:, :], in0=ob[o][:, :], in1=xr[o][:, :],
                                                                      op=ALU.add),
                          reads=[("ob", o), ("xr", o)], writes=[("ob", o)])
                    tk.dma("sp", out[t * 128:(t + 1) * 128, n0c:n0c + NB], ob[o][:, :], reads=[("ob", o)])
        tk.finish()
    return nc


_NC = {}


def _prog(key, fn):
    if key not in _NC:
        _NC[key] = fn()
    return _NC[key]


def _rope_table(hd):
    rows = 4096 // 64
    pos_row = np.repeat(np.arange(rows), 64)
    pos_col = np.tile(np.arange(64), rows)
    q4 = hd // 4
    inv = (10000.0 ** (-np.arange(q4, dtype=np.float32) / q4)).astype(np.float32)
    ang = np.stack([pos_row[:, None].astype(np.float32) * inv, pos_col[:, None].astype(np.float32) * inv],
                   axis=1).astype(np.float32)
    tab = np.zeros((NTOK, 2, hd // 2), np.float32)
    tab[:256, 0] = 1.0
    tab[256:, 0] = np.cos(ang).reshape(4096, -1)
    tab[256:, 1] = np.sin(ang).reshape(4096, -1)
    return tab


def _attn_masks():
    j = np.arange(128)[:, None]
    i = np.arange(128)[None, :]
    A = (j >= i).astype(np.float32)
    B = (j <= i).astype(np.float32)
    return np.stack([np.tile(A, (1, 4)), np.tile(B, (1, 4))])


def _ssd_masks():
    s = np.arange(128)[:, None]
    l = np.arange(128)[None, :]
    NEG = -30000.0
    out = np.zeros((8, 128, 512), np.float32)
    for j in range(4):
        for jj in range(4):
            if jj == j:
                bF = np.where(s <= l, 0.0, NEG)
                bR = np.where(s >= l, 0.0, NEG)
            else:
                bF = np.full((128, 128), NEG if jj < j else 0.0)
                bR = np.full((128, 128), NEG if jj > j else 0.0)
            out[j, :, jj * 128:(jj + 1) * 128] = bF
            out[4 + j, :, jj * 128:(jj + 1) * 128] = bR
    return out


def kernel(x, c, ctx, c_ctx, ada_w, ada_b, norm_mix, norm_ffn,
           swa_w_in, swa_q_norm, swa_k_norm, swa_sink, swa_w_out,
           ssd_w_in, ssd_conv_w, ssd_conv_b, ssd_dt_bias, ssd_a_log, ssd_d, ssd_norm, ssd_w_out,
           ga_w_in, ga_q_norm, ga_k_norm, ga_w_out,
           moe_w_router, moe_b_router, moe_w_gate_up, moe_b_gate_up, moe_w_down, moe_b_down):
    f32 = lambda a: np.ascontiguousarray(np.asarray(a, dtype=np.float32))
    x, c, ctx, c_ctx = f32(x), f32(c), f32(ctx), f32(c_ctx)
    TS = 2176
    X = np.concatenate([ctx, x], axis=1)
    depth = ada_w.shape[0]
    cin = np.zeros((128, D), np.float32)
    cin[:4] = c
    cin[4] = c_ctx
    nc = _prog(("silu",), lambda: build_gemm(128, D, 6144, "silu", bias=True))
    maps = []
    for k in range(8):
        i, hf = k // 2, k % 2
        maps.append({"x": cin, "w": f32(ada_w[i][:, hf * 6144:(hf + 1) * 6144]),
                     "brow": f32(ada_b[i][None, hf * 6144:(hf + 1) * 6144])})
    res = _run(nc, maps)
    mods = [np.concatenate([res[2 * i]["out"][:5], res[2 * i + 1]["out"][:5]], axis=1).reshape(5, 6, D)
            for i in range(depth)]

    def modsets(i, b, s, j):
        lat = mods[i][b, j]
        return (mods[i][4, j] if s == 0 else lat), lat

    def shard(A_, b, s):
        return np.ascontiguousarray(A_[b, s * TS:(s + 1) * TS])

    for i in range(depth):
        kind, j = i % 3, i // 3
        w_in = f32([swa_w_in, ssd_w_in, ga_w_in][kind][j])
        n_in = w_in.shape[1]
        nc = _prog(("norm", n_in), lambda: build_gemm(TS, D, n_in, "norm", n0=2, out_bf16=True))
        maps = []
        for b in range(4):
            for s in range(2):
                sc0, sc1 = modsets(i, b, s, 1)
                sh0, sh1 = modsets(i, b, s, 0)
                g = f32(norm_mix[i])
                AB = np.stack([np.stack([g, sc0, sh0]), np.stack([g, sc1, sh1])]).astype(np.float32)
                maps.append({"x": shard(X, b, s), "AB": AB, "w": w_in})
        res = _run(nc, maps)
        PROJ = np.stack([np.concatenate([res[2 * b]["out"], res[2 * b + 1]["out"]], axis=0) for b in range(4)])
        if kind in (0, 2):
            if kind == 0:
                hd, NQ, NKV, window = 64, 32, 4, True
                qg, kg, sink, w_out = f32(swa_q_norm[j]), f32(swa_k_norm[j]), f32(swa_sink[j]), f32(swa_w_out[j])
            else:
                hd, NQ, NKV, window = 128, 16, 4, False
                qg, kg, sink, w_out = f32(ga_q_norm[j]), f32(ga_k_norm[j]), np.zeros(16, np.float32), f32(ga_w_out[j])
            R = NQ // NKV
            nc = _prog(("attn", hd), lambda: build_attn(hd, R, window))
            tab, am = _rope_table(hd), _attn_masks()
            maps = []
            for b in range(4):
                for hf in range(2):
                    P_ = PROJ[b]
                    maps.append({
                        "q": np.ascontiguousarray(P_[:, hf * 2 * R * hd:(hf + 1) * 2 * R * hd]),
                        "k": np.ascontiguousarray(P_[:, NQ * hd + hf * 2 * hd:NQ * hd + (hf + 1) * 2 * hd]),
                        "v": np.ascontiguousarray(P_[:, (NQ + NKV) * hd + hf * 2 * hd:(NQ + NKV) * hd + (hf + 1) * 2 * hd]),
                        "gains": np.stack([qg, kg]), "cossin": tab, "masks": am,
                        "sink": np.ascontiguousarray(sink[None, hf * 2 * R:(hf + 1) * 2 * R])})
            res = _run(nc, maps)
            YT = [np.concatenate([res[2 * b]["yT"], res[2 * b + 1]["yT"]], axis=0) for b in range(4)]
            nc = _prog(("resid",), lambda: build_gemm(TS, D, D, "resid", n0=2))
            maps = []
            for b in range(4):
                for s in range(2):
                    g0, g1 = modsets(i, b, s, 2)
                    maps.append({"xT": np.ascontiguousarray(YT[b][:, s * TS:(s + 1) * TS]), "xres": shard(X, b, s),
                                 "modrow": np.stack([g0, g1]), "w": w_out})
            res = _run(nc, maps)
            X1 = np.stack([np.concatenate([res[2 * b]["out"], res[2 * b + 1]["out"]], axis=0) for b in range(4)])
        else:
            nc = _prog(("ssd",), lambda: build_ssd())
            cw, cbv = f32(ssd_conv_w[j]), f32(ssd_conv_b[j])
            dtb, alog, dsk = f32(ssd_dt_bias[j]), f32(ssd_a_log[j]), f32(ssd_d[j])
            sm_ = _ssd_masks()
            maps = []
            for b in range(4):
                P_ = PROJ[b].astype(np.float32)
                xbc = P_[:, 4096:10240]
                dtr = P_[:, 10240:10368].reshape(NTOK, 2, 64)
                for hh in range(2):
                    xT = np.ascontiguousarray(xbc[:, hh * 2048:(hh + 1) * 2048].T).reshape(32, 64, NTOK)
                    chB = [4096 + (4 * hh + g) * 128 for g in range(4)]
                    chC = [5120 + (4 * hh + g) * 128 for g in range(4)]
                    bcT = np.ascontiguousarray(np.stack([xbc[:, c0:c0 + 128].T for c0 in chB + chC]))
                    cwx = np.zeros((64, 32, 6), np.float32)
                    for h in range(32):
                        c0 = hh * 2048 + h * 64
                        cwx[:, h, :5] = cw[:, c0:c0 + 64].T
                        cwx[:, h, 5] = cbv[c0:c0 + 64]
                    cwb = np.zeros((128, 8, 6), np.float32)
                    for jj, c0 in enumerate(chB + chC):
                        cwb[:, jj, :5] = cw[:, c0:c0 + 128].T
                        cwb[:, jj, 5] = cbv[c0:c0 + 128]
                    dtT = np.zeros((64, NTOK), np.float32)
                    dpar = np.zeros((64, 2), np.float32)
                    for d_ in range(2):
                        dtT[d_ * 32:(d_ + 1) * 32] = dtr[:, d_, hh * 32:(hh + 1) * 32].T
                        dpar[d_ * 32:(d_ + 1) * 32, 0] = dtb[d_, hh * 32:(hh + 1) * 32]
                        dpar[d_ * 32:(d_ + 1) * 32, 1] = alog[d_, hh * 32:(hh + 1) * 32]
                    dcol = np.ascontiguousarray(np.tile(dsk[None, hh * 32:(hh + 1) * 32], (64, 1)))
                    maps.append({"xT": xT, "bcT": bcT, "cwx": cwx, "cwb": cwb, "dtT": dtT, "dpar": dpar,
                                 "dcol": dcol, "masks": sm_})
            res = _run(nc, maps)
            Y = [np.ascontiguousarray(np.concatenate([res[2 * b]["yT"].T, res[2 * b + 1]["yT"].T], axis=1))
                 for b in range(4)]
            X1 = np.zeros_like(X)
            w_out = f32(ssd_w_out[j])
            gn = f32(ssd_norm[j])[None]
            o = 0
            for ntl in (17,):
                Tp = ntl * 128
                nc = _prog(("ssdfin", Tp), lambda: build_ssdfin(Tp, 2))
                maps = []
                for b in range(4):
                    for s in range(2):
                        g0, g1 = modsets(i, b, s, 2)
                        if o > 0:
                            g0 = g1
                        r0 = s * TS + o
                        maps.append({"y": np.ascontiguousarray(Y[b][r0:r0 + Tp]),
                                     "z": np.ascontiguousarray(PROJ[b][r0:r0 + Tp, :4096].astype(np.float32)), "gn": gn,
                                     "xres": np.ascontiguousarray(X[b, r0:r0 + Tp]), "modrow": np.stack([g0, g1]),
                                     "w": w_out})
                res = _run(nc, maps)
                for b in range(4):
                    for s in range(2):
                        r0 = s * TS + o
                        X1[b, r0:r0 + Tp] = res[2 * b + s]["out"]
                o += Tp
        nc = _prog(("router",), lambda: build_gemm(TS, D, NE, "norm", n0=2, bias=True, emit_hT=True))
        maps = []
        for b in range(4):
            for s in range(2):
                sc0, sc1 = modsets(i, b, s, 4)
                sh0, sh1 = modsets(i, b, s, 3)
                g = f32(norm_ffn[i])
                AB = np.stack([np.stack([g, sc0, sh0]), np.stack([g, sc1, sh1])]).astype(np.float32)
                maps.append({"x": shard(X1, b, s), "AB": AB, "w": f32(moe_w_router[i]),
                             "brow": f32(moe_b_router[i])[None]})
        res = _run(nc, maps)
        HT = np.ascontiguousarray(np.concatenate([res[k]["hTo"] for k in range(8)], axis=1))
        LG = np.concatenate([res[k]["out"] for k in range(8)], axis=0)
        nc = _prog(("moe",), lambda: build_moe_ep(8 * TS, 6))
        maps = []
        for k in range(8):
            es_ = slice(4 * k, 4 * k + 4)
            maps.append({"hT_in": HT, "lg_in": np.ascontiguousarray(np.roll(LG, -4 * k, axis=1)),
                         "wgu": f32(moe_w_gate_up[i][es_]),
                         "bguT": np.ascontiguousarray(f32(moe_b_gate_up[i][es_]).reshape(4, 14, 128).transpose(2, 0, 1)),
                         "wdn": f32(moe_w_down[i][es_]), "bdn": f32(moe_b_down[i][es_])})
        res = _run(nc, maps)
        parts = [res[k]["out"] for k in range(8)]
        nc = _prog(("combine",), lambda: build_combine(TS, 2))
        maps = []
        for b in range(4):
            for s in range(2):
                cidx = 2 * b + s
                g0, g1 = modsets(i, b, s, 5)
                maps.append({"parts": np.ascontiguousarray(np.stack([p[cidx * TS:(cidx + 1) * TS] for p in parts])),
                             "x1": shard(X1, b, s), "modrow": np.stack([g0, g1])})
        res = _run(nc, maps)
        X = np.stack([np.concatenate([res[2 * b]["out"], res[2 * b + 1]["out"]], axis=0) for b in range(4)])
    return np.ascontiguousarray(X[:, 256:, :]).astype(np.float32)
```
